# Optimizing a Trainium2 kernel written in Bass

```python
import math
import jax
import jax.numpy as jnp
from jax import lax
import numpy as np

D_MODEL = 1024
BATCH = 16
SEQ = 2048
DEPTH = 1

CHUNK = 64
EPS = 1e-6

ATTN_HEAD_DIM = 64
ATTN_WIDTH = D_MODEL // 2
ATTN_HEADS = ATTN_WIDTH // ATTN_HEAD_DIM
LEFT_CHUNKS = 8
BAND = (LEFT_CHUNKS + 1) * CHUNK
REL_CLIP = 128
N_REL = 2 * REL_CLIP + 1

DN_HEAD_DIM = 128
DN_WIDTH = D_MODEL - ATTN_WIDTH
DN_HEADS = DN_WIDTH // DN_HEAD_DIM
CONV_WIDTH = 4

MIX_WIDTH = ATTN_WIDTH + DN_WIDTH
IN_PROJ_WIDTH = 3 * ATTN_WIDTH + 4 * DN_WIDTH + 2 * DN_HEADS

PEER_HEADS = 8
PEER_KEY_DIM = 256
PEER_HALF = PEER_KEY_DIM // 2
N_KEYS = 128
N_EXPERTS = N_KEYS * N_KEYS
PEER_TOPK = 16
PEER_BLOCK = 128

kernel_name = "hymba_chunkattn_gdn_peer_adaln"


def rms_norm(x, gain):
    xf = x.astype(jnp.float32)
    y = xf * lax.rsqrt(jnp.mean(xf * xf, axis=-1, keepdims=True) + EPS)
    return (y * gain.astype(jnp.float32)).astype(x.dtype)


def l2_norm(x):
    xf = x.astype(jnp.float32)
    return xf * lax.rsqrt(jnp.sum(xf * xf, axis=-1, keepdims=True) + EPS)


def modulate(h, shift, scale):
    return h * (1.0 + scale) + shift


def chunked_band_attention(q, k, v, q_gain, k_gain, rel_bias):
    B, S = q.shape[:2]
    nc = S // CHUNK
    shp = (B, nc, CHUNK, ATTN_HEADS, ATTN_HEAD_DIM)
    pad = ((0, 0), (LEFT_CHUNKS, 0), (0, 0), (0, 0), (0, 0))
    q = rms_norm(q, q_gain).reshape(shp)
    k = jnp.pad(rms_norm(k, k_gain).reshape(shp), pad)
    v = jnp.pad(v.reshape(shp), pad)
    band_idx = np.arange(nc)[:, None] + np.arange(LEFT_CHUNKS + 1)[None, :]
    kb = k[:, band_idx].reshape(B, nc, BAND, ATTN_HEADS, ATTN_HEAD_DIM)
    vb = v[:, band_idx].reshape(B, nc, BAND, ATTN_HEADS, ATTN_HEAD_DIM)
    scores = jnp.einsum("bnqhd,bnkhd->bhnqk", q, kb,
                        preferred_element_type=jnp.float32) * (ATTN_HEAD_DIM ** -0.5)
    rel = np.arange(CHUNK)[:, None] + LEFT_CHUNKS * CHUNK - np.arange(BAND)[None, :]
    rel_idx = np.clip(rel, -REL_CLIP, REL_CLIP) + REL_CLIP
    bias = rel_bias.astype(jnp.float32)[:, rel_idx]
    valid = (np.arange(nc)[:, None] + np.arange(BAND)[None, :] // CHUNK) >= LEFT_CHUNKS
    scores = jnp.where(valid[None, None, :, None, :], scores + bias[None, :, None], -jnp.inf)
    p = jax.nn.softmax(scores, axis=-1).astype(vb.dtype)
    o = jnp.einsum("bhnqk,bnkhd->bnqhd", p, vb)
    return o.reshape(B, S, ATTN_WIDTH)


def causal_depthwise_conv(x, w):
    S = x.shape[1]
    xp = jnp.pad(x, ((0, 0), (CONV_WIDTH - 1, 0), (0, 0)))
    y = xp[:, 0:S] * w[0]
    for i in range(1, CONV_WIDTH):
        y = y + xp[:, i:i + S] * w[i]
    return jax.nn.silu(y)


def gated_delta_rule(q, k, v, g, beta):
    B, S, H, Dk = q.shape
    Dv = v.shape[-1]
    nc = S // CHUNK

    def to_chunks(t):
        return t.astype(jnp.float32).reshape(B, nc, CHUNK, H, -1).transpose(1, 0, 3, 2, 4)

    q = to_chunks(q) * (Dk ** -0.5)
    k = to_chunks(k)
    v = to_chunks(v)
    g = g.astype(jnp.float32).reshape(B, nc, CHUNK, H).transpose(1, 0, 3, 2)
    beta = beta.astype(jnp.float32).reshape(B, nc, CHUNK, H).transpose(1, 0, 3, 2)
    gc = jnp.cumsum(g, axis=-1)
    tri = np.tril(np.ones((CHUNK, CHUNK), dtype=bool))
    strict = np.tril(np.ones((CHUNK, CHUNK), dtype=bool), -1)
    decay = jnp.exp(jnp.where(tri, gc[..., :, None] - gc[..., None, :], -jnp.inf))
    k_beta = k * beta[..., None]
    v_beta = v * beta[..., None]
    lower = jnp.where(strict, jnp.einsum("nbhid,nbhjd->nbhij", k_beta, k) * decay, 0.0)
    rhs = jnp.concatenate([v_beta, k_beta * jnp.exp(gc)[..., None]], axis=-1)
    sol = lax.linalg.triangular_solve(jnp.eye(CHUNK, dtype=jnp.float32) + lower, rhs,
                                      left_side=True, lower=True, unit_diagonal=True)
    u = sol[..., :Dv]
    w = sol[..., Dv:]
    intra = jnp.einsum("nbhid,nbhjd->nbhij", q, k) * decay

    def step(state, xs):
        q_c, k_c, u_c, w_c, a_c, g_c = xs
        v_new = u_c - jnp.einsum("bhcd,bhde->bhce", w_c, state)
        o = (jnp.einsum("bhcd,bhde->bhce", q_c * jnp.exp(g_c)[..., None], state)
             + jnp.einsum("bhij,bhje->bhie", a_c, v_new))
        g_last = g_c[..., -1]
        k_dec = k_c * jnp.exp(g_last[..., None] - g_c)[..., None]
        state = state * jnp.exp(g_last)[..., None, None] + jnp.einsum("bhcd,bhce->bhde", k_dec, v_new)
        return state, o

    state0 = jnp.zeros((B, H, Dk, Dv), jnp.float32)
    _, o = lax.scan(step, state0, (q, k, u, w, intra, gc))
    return o.transpose(1, 0, 3, 2, 4).reshape(B, S, H, Dv)


def peer(h, w_query, query_gain, sub_keys_1, sub_keys_2, expert_down, expert_up):
    B, S, D = h.shape
    n_blocks = (B * S) // PEER_BLOCK

    def block(hb):
        qry = (hb @ w_query).reshape(PEER_BLOCK, PEER_HEADS, PEER_KEY_DIM)
        qry = rms_norm(qry, query_gain)
        s1 = jnp.einsum("thd,nd->thn", qry[..., :PEER_HALF], sub_keys_1, preferred_element_type=jnp.float32)
        s2 = jnp.einsum("thd,nd->thn", qry[..., PEER_HALF:], sub_keys_2, preferred_element_type=jnp.float32)
        v1, i1 = lax.top_k(s1, PEER_TOPK)
        v2, i2 = lax.top_k(s2, PEER_TOPK)
        cand_s = (v1[..., :, None] + v2[..., None, :]).reshape(PEER_BLOCK, PEER_HEADS, PEER_TOPK * PEER_TOPK)
        cand_i = (i1[..., :, None] * N_KEYS + i2[..., None, :]).reshape(PEER_BLOCK, PEER_HEADS, PEER_TOPK * PEER_TOPK)
        top_s, pos = lax.top_k(cand_s, PEER_TOPK)
        experts = jnp.take_along_axis(cand_i, pos, axis=-1)
        gates = jax.nn.softmax(top_s, axis=-1)
        u = expert_down[experts]
        vv = expert_up[experts]
        act = jax.nn.gelu(jnp.einsum("thkd,td->thk", u, hb, preferred_element_type=jnp.float32),
                          approximate=False)
        return jnp.einsum("thk,thkd->td", (gates * act).astype(vv.dtype), vv)

    out = lax.map(block, h.reshape(n_blocks, PEER_BLOCK, D))
    return out.reshape(B, S, D)


def hybrid_layer(x, c, w_ada, b_ada, norm1_gain, w_in, attn_q_gain, attn_k_gain, attn_rel_bias,
                 attn_out_gain, dn_conv_w, dn_a_log, dn_dt_bias, dn_out_gain, w_out, norm2_gain,
                 peer_w_query, peer_query_gain, peer_sub_keys_1, peer_sub_keys_2,
                 peer_expert_down, peer_expert_up):
    B, S, _ = x.shape
    mod = jax.nn.silu(c) @ w_ada + b_ada
    shift1, scale1, gate1, shift2, scale2, gate2 = [m[:, None, :] for m in jnp.split(mod, 6, axis=-1)]

    h = modulate(rms_norm(x, norm1_gain), shift1, scale1)
    proj = h @ w_in
    cuts = [ATTN_WIDTH, 2 * ATTN_WIDTH, 3 * ATTN_WIDTH, 3 * ATTN_WIDTH + 3 * DN_WIDTH,
            3 * ATTN_WIDTH + 4 * DN_WIDTH, 3 * ATTN_WIDTH + 4 * DN_WIDTH + DN_HEADS]
    qa, ka, va, qkv_b, z_b, b_raw, a_raw = jnp.split(proj, cuts, axis=-1)

    hd_a = (B, S, ATTN_HEADS, ATTN_HEAD_DIM)
    o_a = chunked_band_attention(qa.reshape(hd_a), ka.reshape(hd_a), va.reshape(hd_a),
                                 attn_q_gain, attn_k_gain, attn_rel_bias)
    o_a = rms_norm(o_a, attn_out_gain)

    qkv_b = causal_depthwise_conv(qkv_b, dn_conv_w)
    q_b, k_b, v_b = jnp.split(qkv_b, 3, axis=-1)
    hd_b = (B, S, DN_HEADS, DN_HEAD_DIM)
    q_b = l2_norm(q_b.reshape(hd_b))
    k_b = l2_norm(k_b.reshape(hd_b))
    beta = jax.nn.sigmoid(b_raw.astype(jnp.float32))
    g = -jnp.exp(dn_a_log.astype(jnp.float32)) * jax.nn.softplus(
        a_raw.astype(jnp.float32) + dn_dt_bias.astype(jnp.float32))
    o_b = gated_delta_rule(q_b, k_b, v_b.reshape(hd_b), g, beta)
    o_b = rms_norm(o_b, dn_out_gain) * jax.nn.silu(z_b.reshape(hd_b).astype(jnp.float32))
    o_b = o_b.reshape(B, S, DN_WIDTH).astype(x.dtype)

    mix = jnp.concatenate([o_a, o_b], axis=-1) @ w_out
    x = x + gate1 * mix

    h2 = modulate(rms_norm(x, norm2_gain), shift2, scale2)
    y = peer(h2, peer_w_query, peer_query_gain, peer_sub_keys_1, peer_sub_keys_2,
             peer_expert_down, peer_expert_up)
    return x + gate2 * y


def setup_inputs(seed: int = 0) -> dict:
    key = jax.random.key(seed)
    ks = iter(jax.random.split(key, 32))
    L = DEPTH

    def nrm(shape, scale):
        return scale * jax.random.normal(next(ks), shape, jnp.float32)

    def gain(shape):
        return 1.0 + nrm(shape, 0.02)

    x = nrm((BATCH, SEQ, D_MODEL), 1.0)
    c = nrm((BATCH, D_MODEL), 1.0)
    w_ada = nrm((L, D_MODEL, 6 * D_MODEL), 0.5 * D_MODEL ** -0.5)
    b_ada = nrm((L, 6 * D_MODEL), 0.02)
    norm1_gain = gain((L, D_MODEL))
    w_in = nrm((L, D_MODEL, IN_PROJ_WIDTH), D_MODEL ** -0.5)
    attn_q_gain = gain((L, ATTN_HEAD_DIM))
    attn_k_gain = gain((L, ATTN_HEAD_DIM))
    attn_rel_bias = nrm((L, ATTN_HEADS, N_REL), 0.5)
    attn_out_gain = gain((L, ATTN_WIDTH))
    dn_conv_w = nrm((L, CONV_WIDTH, 3 * DN_WIDTH), CONV_WIDTH ** -0.5)
    dn_a_log = jnp.log(jax.random.uniform(next(ks), (L, DN_HEADS), jnp.float32, 1.0, 16.0))
    dt = jnp.exp(jax.random.uniform(next(ks), (L, DN_HEADS), jnp.float32,
                                    math.log(1e-3), math.log(1e-1)))
    dn_dt_bias = dt + jnp.log(-jnp.expm1(-dt))
    dn_out_gain = gain((L, DN_HEAD_DIM))
    w_out = nrm((L, MIX_WIDTH, D_MODEL), MIX_WIDTH ** -0.5)
    norm2_gain = gain((L, D_MODEL))
    peer_w_query = nrm((L, D_MODEL, PEER_HEADS * PEER_KEY_DIM), D_MODEL ** -0.5)
    peer_query_gain = gain((L, PEER_KEY_DIM))
    peer_sub_keys_1 = nrm((L, N_KEYS, PEER_HALF), PEER_HALF ** -0.5)
    peer_sub_keys_2 = nrm((L, N_KEYS, PEER_HALF), PEER_HALF ** -0.5)
    peer_expert_down = nrm((L, N_EXPERTS, D_MODEL), D_MODEL ** -0.5)
    peer_expert_up = nrm((L, N_EXPERTS, D_MODEL), 0.5)
    return {"x": x, "c": c, "w_ada": w_ada, "b_ada": b_ada, "norm1_gain": norm1_gain,
            "w_in": w_in, "attn_q_gain": attn_q_gain, "attn_k_gain": attn_k_gain,
            "attn_rel_bias": attn_rel_bias, "attn_out_gain": attn_out_gain,
            "dn_conv_w": dn_conv_w, "dn_a_log": dn_a_log, "dn_dt_bias": dn_dt_bias,
            "dn_out_gain": dn_out_gain, "w_out": w_out, "norm2_gain": norm2_gain,
            "peer_w_query": peer_w_query, "peer_query_gain": peer_query_gain,
            "peer_sub_keys_1": peer_sub_keys_1, "peer_sub_keys_2": peer_sub_keys_2,
            "peer_expert_down": peer_expert_down, "peer_expert_up": peer_expert_up}


def reference(x, c, w_ada, b_ada, norm1_gain, w_in, attn_q_gain, attn_k_gain, attn_rel_bias,
              attn_out_gain, dn_conv_w, dn_a_log, dn_dt_bias, dn_out_gain, w_out, norm2_gain,
              peer_w_query, peer_query_gain, peer_sub_keys_1, peer_sub_keys_2,
              peer_expert_down, peer_expert_up):
    for layer in range(DEPTH):
        x = hybrid_layer(x, c, w_ada[layer], b_ada[layer], norm1_gain[layer], w_in[layer],
                         attn_q_gain[layer], attn_k_gain[layer], attn_rel_bias[layer],
                         attn_out_gain[layer], dn_conv_w[layer], dn_a_log[layer], dn_dt_bias[layer],
                         dn_out_gain[layer], w_out[layer], norm2_gain[layer], peer_w_query[layer],
                         peer_query_gain[layer], peer_sub_keys_1[layer], peer_sub_keys_2[layer],
                         peer_expert_down[layer], peer_expert_up[layer])
    return x
```

```python
from contextlib import ExitStack
import math
import numpy as np
import concourse.bass as bass
import concourse.mybir as mybir
from concourse.bass_utils import run_bass_kernel_spmd

F32 = mybir.dt.float32
BF16 = mybir.dt.bfloat16
I32 = mybir.dt.int32
U32 = mybir.dt.uint32
ALU = mybir.AluOpType
AF = mybir.ActivationFunctionType
AX = mybir.AxisListType

N_DMA_SEMS = 12
import os
KSTOP = int(os.environ.get('KSTOP', '99'))


class _Stop(Exception):
    pass


def chk(n):
    if KSTOP == n:
        raise _Stop()
EPS = 1e-6


class Sched:
    def __init__(self, nc, es):
        self.nc = nc
        self.eng = {"pe": nc.tensor, "act": nc.scalar, "dve": nc.vector,
                    "pool": nc.gpsimd, "sp": nc.sync}
        self.sem = {k: es.enter_context(nc.semaphore("s_" + k)) for k in self.eng}
        self.cnt = {k: 0 for k in self.eng}
        self.seen = {k: {} for k in self.eng}
        self.dsem = [es.enter_context(nc.semaphore("d%d" % i)) for i in range(N_DMA_SEMS)]
        self.dval = [0] * N_DMA_SEMS
        self.dnext = 0
        self.last_w = {}
        self.readers = {}
        self.n_wait = 0
        self.n_inst = 0

    def _wait(self, e, tok):
        kind, key, val = tok
        if kind == "e" and key == "pe" and e == "pe":
            return
        skey = (kind, key)
        if self.seen[e].get(skey, 0) >= val:
            return
        sem = self.sem[key] if kind == "e" else self.dsem[key]
        self.eng[e].wait_ge(sem, val)
        self.seen[e][skey] = val
        self.n_wait += 1

    def _deps(self, e, reads, writes):
        for b in reads:
            t = self.last_w.get(b)
            if t is not None:
                self._wait(e, t)
        for b in writes:
            t = self.last_w.get(b)
            if t is not None:
                self._wait(e, t)
            for t in self.readers.get(b, ()):
                self._wait(e, t)

    def _commit(self, tok, reads, writes):
        for b in reads:
            lst = self.readers.setdefault(b, [])
            lst.append(tok)
            if len(lst) > 16:
                newest = {}
                for t in lst:
                    k = (t[0], t[1])
                    if k not in newest or newest[k][2] < t[2]:
                        newest[k] = t
                self.readers[b] = list(newest.values())
        for b in writes:
            self.last_w[b] = tok
            self.readers[b] = []

    def op(self, e, fn, reads=(), writes=()):
        if e != "pe":
            extra = [r for r in reads if r.startswith("pb") or r.startswith("pq")]
            if extra:
                writes = list(writes) + extra
        self._deps(e, reads, writes)
        inst = fn(self.eng[e])
        self.cnt[e] += 1
        inst.then_inc(self.sem[e], 1)
        tok = ("e", e, self.cnt[e])
        self._commit(tok, reads, writes)
        self.n_inst += 1
        return tok

    def dma(self, q, fn, reads=(), writes=()):
        self._deps(q, reads, writes)
        i = self.dnext
        self.dnext = (self.dnext + 1) % N_DMA_SEMS
        if self.dval[i] > 0:
            self._wait(q, ("d", i, self.dval[i]))
        inst = fn(self.eng[q])
        self.dval[i] += 16
        inst.then_inc(self.dsem[i], 16)
        tok = ("d", i, self.dval[i])
        self._commit(tok, reads, writes)
        self.n_inst += 1
        return tok

    def barrier(self, engines=("pe", "act", "dve", "pool", "sp")):
        for e in engines:
            for k in self.eng:
                if self.cnt[k] and k != e:
                    self._wait(e, ("e", k, self.cnt[k]))
            for i in range(N_DMA_SEMS):
                if self.dval[i]:
                    self._wait(e, ("d", i, self.dval[i]))


def cv(ap, dims):
    return bass.AP(ap.tensor, ap.offset, [list(ap.ap[0])] + [list(d) for d in dims])


C_IDENT, C_TRI, C_TRIT, C_STRI, C_SELT, C_SEL0, C_SEL1, C_BLK64, C_ONES = range(9)
NCST = 9


def make_consts():
    c = np.zeros((128, NCST, 128), np.float32)
    i = np.arange(128)[:, None]
    j = np.arange(128)[None, :]
    same = (i // 64) == (j // 64)
    c[:, C_IDENT] = (i == j)
    c[:, C_TRI] = same & (j <= i)
    c[:, C_TRIT] = same & (i <= j)
    c[:, C_STRI] = same & (j < i)
    c[:, C_SELT] = (i == 64 * (j // 64) + 63)
    c[:, C_SEL0] = (i == 63)
    c[:, C_SEL1] = (i == 127)
    c[:, C_BLK64] = same / 64.0
    c[:, C_ONES] = 1.0
    return c


def build_program(NTILES=32, dbg=False, do_peer=True):
    nc = bass.Bass("TRN2", target_bir_lowering=False)
    D = {}

    def din(name, shape, dt=F32):
        D[name] = nc.dram_tensor(name, shape, dt, kind="ExternalInput").ap()

    din("x", [4096, 1024]); din("cT", [128, 8, 2]); din("w_ada", [1024, 6144])
    din("b_adaT", [128, 48]); din("b_gate", [128, 2, 1024]); din("n1g", [128, 8]); din("n2g", [128, 8])
    din("w_in", [1024, 3592]); din("qkgain", [128, 2]); din("biasT", [128, 8, 2, 128])
    din("cbias", [128, 8]); din("aog", [128, 512]); din("convw", [128, 12, 4])
    din("alog", [128, 4]); din("dtb", [128, 4]); din("dog", [128, 128])
    din("w_out", [1024, 1024]); din("wq", [1024, 2048]); din("qg", [128, 256])
    din("sk1T", [128, 128]); din("sk2T", [128, 128])
    din("tabf", [16384, 2048]); din("iota16", [128, 16]); din("cst", [128, NCST, 128])
    out = nc.dram_tensor("out", [4096, 1024], F32, kind="ExternalOutput").ap()
    tab = nc.dram_tensor("tab", [16384, 2048], BF16, kind="Internal").ap()
    if dbg:
        dbg_o = nc.dram_tensor("dbg", [4096, 1024], F32, kind="ExternalOutput").ap()

    with ExitStack() as es:
        S = Sched(nc, es)
        uid = [0]

        def sb(es_, name, shape, dt=F32):
            uid[0] += 1
            return es_.enter_context(nc.sbuf_tensor("%s_%d" % (name, uid[0]), shape, dt))

        PB = [es.enter_context(nc.psum_tensor("pb%d" % i, [128, 512], F32)) for i in range(8)]
        bank_rr = [0]

        def bank():
            i = bank_rr[0]
            bank_rr[0] = (i + 1) % 4
            return PB[i], "pb%d" % i

        q_rr = [0]

        def quarter():
            i = q_rr[0]
            q_rr[0] = (i + 1) % 6
            bi = (0, 1, 2, 3, 6, 7)[i]
            return PB[bi][:, 0:128], "pb%d" % bi

        cst = sb(es, "cst", [128, NCST, 128])
        S.dma("sp", lambda e: e.dma_start(out=cst[:], in_=D["cst"]), writes=["cst"])

        def C(i):
            return cst[:, i, :]
        identb = sb(es, "identb", [128, 128], BF16)
        S.op("dve", lambda e: e.tensor_copy(out=identb[:], in_=C(C_IDENT)), reads=["cst"], writes=["identb"])

        w_in_sb = sb(es, "w_in_sb", [128, 8, 3592], BF16)
        W_IN = []
        for k in range(8):
            for (c0, c1) in ((0, 2048), (2048, 3592)):
                nm = "w_in_%d_%d" % (k, c0)
                W_IN.append(nm)
                S.dma("pool", lambda e: e.dma_start(out=w_in_sb[:, k, c0:c1], in_=D["w_in"][k * 128:(k + 1) * 128, c0:c1]),
                      writes=[nm])
        w_out_sb = sb(es, "w_out_sb", [128, 8, 1024], BF16)
        wq_sb = sb(es, "wq_sb", [128, 8, 2048], BF16)
        W_OUT, W_Q = [], []
        for k in range(8):
            nm = "w_out_%d" % k
            W_OUT.append(nm)
            S.dma("pool", lambda e: e.dma_start(out=w_out_sb[:, k, :], in_=D["w_out"][k * 128:(k + 1) * 128, :]), writes=[nm])
            nm = "wq_%d" % k
            W_Q.append(nm)
            S.dma("pool", lambda e: e.dma_start(out=wq_sb[:, k, :], in_=D["wq"][k * 128:(k + 1) * 128, :]), writes=[nm])
        skT = sb(es, "skT", [128, 2, 128], BF16)
        S.dma("pool", lambda e: e.dma_start(out=skT[:, 0, :], in_=D["sk1T"]), writes=["sk1"])
        S.dma("pool", lambda e: e.dma_start(out=skT[:, 1, :], in_=D["sk2T"]), writes=["sk2"])

        small = sb(es, "small", [128, 2048])
        off = {}
        pos = [0]

        def sm_load(name, n, src, q="sp"):
            o = pos[0]
            off[name] = o
            pos[0] += n
            S.dma(q, lambda e: e.dma_start(out=small[:, o:o + n], in_=src), writes=["sm_" + name])
            return small[:, o:o + n]

        def sm_alloc(name, n):
            o = pos[0]
            off[name] = o
            pos[0] += n
            return small[:, o:o + n]

        b_adaT = sm_load("b_adaT", 48, D["b_adaT"])
        n1g = sm_load("n1g", 8, D["n1g"])
        n2g = sm_load("n2g", 8, D["n2g"])
        qkgain = sm_load("qkgain", 2, D["qkgain"])
        cbias = sm_load("cbias", 8, D["cbias"])
        convw = sm_load("convw", 48, D["convw"].rearrange("p a b -> p (a b)"))
        alog = sm_load("alog", 4, D["alog"])
        dtb = sm_load("dtb", 4, D["dtb"])
        dog = sm_load("dog", 128, D["dog"])
        qg = sm_load("qg", 256, D["qg"])
        aog = sm_load("aog", 512, D["aog"])
        iota16 = sm_load("iota16", 16, D["iota16"])
        nexpA = sm_alloc("nexpA", 4)
        modT = sm_alloc("modT", 96)
        A1 = sm_alloc("A1", 16); A2 = sm_alloc("A2", 16)
        gq = sm_alloc("gq", 1)
        assert pos[0] <= 2048
        S.op("act", lambda e: e.activation(out=nexpA, in_=alog, func=AF.Exp), reads=["sm_alog"], writes=["nexpA"])
        S.op("dve", lambda e: e.tensor_scalar(out=nexpA, in0=nexpA, scalar1=-1.0, scalar2=None, op0=ALU.mult),
             reads=["nexpA"], writes=["nexpA"])
        S.op("dve", lambda e: e.tensor_scalar(out=gq, in0=qkgain[:, 0:1], scalar1=0.125, scalar2=None, op0=ALU.mult),
             reads=["sm_qkgain"], writes=["gq"])

        expB = sb(es, "expB", [128, 8, 2, 128], BF16)
        gate_rows = sb(es, "gate_rows", [128, 2, 1024])
        csT = sb(es, "csT", [128, 8, 2])
        S.dma("sp", lambda e: e.dma_start(out=csT[:], in_=D["cT"]), writes=["csT"])
        S.op("act", lambda e: e.activation(out=csT[:], in_=csT[:], func=AF.Silu), reads=["csT"], writes=["csT"])

        x_sb = sb(es, "x_sb", [128, 1024])
        kT_ring = sb(es, "kT_ring", [128, 4, 640], BF16)
        V_ring = sb(es, "V_ring", [128, 5, 8, 65], BF16)
        S.op("pool", lambda e: e.memset(V_ring[:], 1.0), writes=["V_ring"])
        craw = sb(es, "craw", [128, 12, 131])
        Sst = sb(es, "Sst", [128, 4, 128])

        with ExitStack() as es0:
            bT = sb(es0, "bT", [128, 8 * 2 * 128])
            S.dma("sp", lambda e: e.dma_start(out=bT[:], in_=D["biasT"].rearrange("p a b c -> p (a b c)")), writes=["bT"])
            S.op("act", lambda e: e.activation(out=expB[:].rearrange("p a b c -> p (a b c)"), in_=bT[:], func=AF.Exp),
                 reads=["bT"], writes=["expB"])
            S.op("pool", lambda e: e.memset(expB[64:128, :, 0, 0:64], 0.0), reads=["expB"], writes=["expB"])

            wab = [sb(es0, "wab%d" % i, [128, 8, 512]) for i in range(2)]
            for blk in range(12):
                wb = wab[blk % 2]
                nm = "wab%d" % (blk % 2)
                S.dma("sp", lambda e: e.dma_start(out=wb[:], in_=D["w_ada"].rearrange("(k p) n -> p k n", p=128)[:, :, blk * 512:(blk + 1) * 512]),
                      writes=[nm])
                pq, pqn = quarter()
                for c4 in range(4):
                    for k in range(8):
                        S.op("pe", lambda e: e.matmul(pq[:, c4 * 2:c4 * 2 + 2], lhsT=wb[:, k, c4 * 128:(c4 + 1) * 128], rhs=csT[:, k, :],
                                                      start=(k == 0), stop=(k == 7)), reads=[nm, "csT"], writes=[pqn])
                o = off["modT"] + blk * 8
                S.op("dve", lambda e: e.tensor_tensor(out=cv(small[:, o:o + 1], [[2, 4], [1, 2]]),
                                                      in0=cv(pq[:, 0:1], [[2, 4], [1, 2]]),
                                                      in1=cv(small[:, off["b_adaT"] + blk * 4:off["b_adaT"] + blk * 4 + 1], [[1, 4], [0, 2]]),
                                                      op=ALU.add), reads=[pqn, "sm_b_adaT"], writes=["modT"])
            for (An, Anm, gn, gnm, ch0) in ((A1, "A1", n1g, "sm_n1g", 8), (A2, "A2", n2g, "sm_n2g", 32)):
                o = off["modT"] + ch0 * 2
                S.op("dve", lambda e: e.scalar_tensor_tensor(out=cv(An[:, 0:1], [[2, 8], [1, 2]]), in0=cv(small[:, o:o + 1], [[2, 8], [1, 2]]),
                                                             scalar=1.0, in1=cv(gn[:, 0:1], [[1, 8], [0, 2]]), op0=ALU.add, op1=ALU.mult),
                     reads=["modT", gnm], writes=[Anm])
            S.barrier()

        with ExitStack() as esc:
            cvb = [sb(esc, "cvb%d" % i, [128, 2, 2048], BF16) for i in range(4)]
            tabv = tab.rearrange("(p r) n -> p r n", p=128)
            srcv = D["tabf"].rearrange("(p r) n -> p r n", p=128)
            for c in range(64):
                cb_ = cvb[c % 4]; cn = "cvb%d" % (c % 4)
                S.dma("pool", lambda e: e.dma_start(out=cb_[:], in_=srcv[:, c * 2:(c + 1) * 2, :]), writes=[cn])
                S.dma("sp", lambda e: e.dma_start(out=tabv[:, c * 2:(c + 1) * 2, :], in_=cb_[:]), reads=[cn])
            S.barrier()

        def modv(ch, b):
            o = off["modT"] + ch * 2 + b
            return small[:, o:o + 1]

        def emit_gate_rows(b):
            with ExitStack() as esg:
                wab = [sb(esg, "wabg%d" % i, [128, 8, 512]) for i in range(2)]
                cb = sb(esg, "cb", [128, 8, 128])
                S.dma("sp", lambda e: e.dma_start(out=gate_rows[:], in_=D["b_gate"]), writes=["gate_rows"])
                for k in range(8):
                    S.op("dve", lambda e: e.tensor_copy(out=cb[:, k, :], in_=cv(csT[:, k, b:b + 1], [[0, 128]])), reads=["csT"], writes=["cb"])
                for gi, blk0 in ((0, 4), (1, 10)):
                    for hb in range(2):
                        blk = blk0 + hb
                        wb = wab[hb]
                        nm = "wabg%d" % hb
                        S.dma("sp", lambda e: e.dma_start(out=wb[:], in_=D["w_ada"].rearrange("(k p) n -> p k n", p=128)[:, :, blk * 512:(blk + 1) * 512]),
                              writes=[nm])
                        pb, pbn = bank()
                        for k in range(8):
                            S.op("pe", lambda e: e.matmul(pb[:, :], lhsT=cb[:, k, :], rhs=wb[:, k, :], start=(k == 0), stop=(k == 7)),
                                 reads=[nm, "cb"], writes=[pbn])
                        S.op("dve", lambda e: e.tensor_tensor(out=gate_rows[:, gi, hb * 512:(hb + 1) * 512], in0=pb[:, :],
                                                              in1=gate_rows[:, gi, hb * 512:(hb + 1) * 512], op=ALU.add),
                             reads=[pbn, "gate_rows"], writes=["gate_rows"])
                S.barrier()

        for ti in range(NTILES):
            b = ti // 16
            it = ti % 16
            if it == 0:
                emit_gate_rows(b)
                S.op("pool", lambda e: e.memset(Sst[:], 0.0), writes=["Sst0", "Sst1", "Sst2", "Sst3"])
            slot = it % 5
            S.dma("sp", lambda e: e.dma_start(out=x_sb[:], in_=D["x"][ti * 128:(ti + 1) * 128, :]), writes=["x"])
            if True:
              def mixer(em):
                _mixer_body = True
                xn = sb(em, "xn", [128, 1024])
                o_cat = sb(em, "o_cat", [128, 1024])
                st = sb(em, "st", [128, 64])
                hT = sb(em, "hT", [128, 8, 128], BF16)
                S.op("act", lambda e: e.activation(out=o_cat[:], in_=x_sb[:], func=AF.Square, accum_out=st[:, 0:1]), reads=["x"], writes=["ocs", "ss"])
                S.op("dve", lambda e: e.tensor_scalar(out=st[:, 1:2], in0=st[:, 0:1], scalar1=1.0 / 1024, scalar2=EPS, op0=ALU.mult, op1=ALU.add),
                     reads=["ss"], writes=["rs"])
                S.op("act", lambda e: e.activation(out=st[:, 1:2], in_=st[:, 1:2], func=AF.Sqrt), reads=["rs"], writes=["rs"])
                S.op("dve", lambda e: e.reciprocal(out=st[:, 1:2], in_=st[:, 1:2]), reads=["rs"], writes=["rs"])
                S.op("act", lambda e: e.activation(out=xn[:], in_=x_sb[:], func=AF.Copy, scale=st[:, 1:2]), reads=["x", "rs"], writes=["xn"])
                for half in range(2):
                    pb, pbn = bank()
                    for kk in range(4):
                        k = half * 4 + kk
                        S.op("pe", lambda e: e.transpose(out=pb[:, kk * 128:(kk + 1) * 128], in_=xn[:, k * 128:(k + 1) * 128], identity=C(C_IDENT)),
                             reads=["xn", "cst"], writes=[pbn])
                    for kk in range(4):
                        k = half * 4 + kk
                        a_ap = small[:, off["A1"] + k * 2 + b:off["A1"] + k * 2 + b + 1]
                        s_ap = modv(k, b)
                        if kk % 2 == 0:
                            S.op("dve", lambda e: e.tensor_scalar(out=hT[:, k, :], in0=pb[:, kk * 128:(kk + 1) * 128], scalar1=a_ap, scalar2=s_ap,
                                                                  op0=ALU.mult, op1=ALU.add), reads=[pbn, "A1", "modT"], writes=["hT"])
                        else:
                            S.op("act", lambda e: e.activation(out=hT[:, k, :], in_=pb[:, kk * 128:(kk + 1) * 128], func=AF.Identity, scale=a_ap, bias=s_ap),
                                 reads=[pbn, "A1", "modT"], writes=["hT"])

                chk(1)
                def proj_fm(pb, pbn, col0, nch):
                    for c4 in range(nch):
                        for k in range(8):
                            S.op("pe", lambda e: e.matmul(pb[:, c4 * 128:(c4 + 1) * 128], lhsT=w_in_sb[:, k, col0 + c4 * 128:col0 + (c4 + 1) * 128],
                                                          rhs=hT[:, k, :], start=(k == 0), stop=(k == 7)), reads=W_IN + ["hT"], writes=[pbn])

                def proj_tm(pb, pbn, col0, n):
                    for k in range(8):
                        S.op("pe", lambda e: e.matmul(pb[:, 0:n], lhsT=hT[:, k, :], rhs=w_in_sb[:, k, col0:col0 + n], start=(k == 0), stop=(k == 7)),
                             reads=W_IN + ["hT"], writes=[pbn])

                sqb = sb(em, "sqb", [128, 512])
                rb = sb(em, "rb", [128, 512])
                qT = sb(em, "qT", [128, 4, 128], BF16)
                for which in range(2):
                    pb, pbn = bank()
                    proj_fm(pb, pbn, which * 512, 4)
                    S.op("act", lambda e: e.activation(out=sqb[:], in_=pb[:, :], func=AF.Square), reads=[pbn], writes=["sqb"])
                    pm, pmn = bank()
                    S.op("pe", lambda e: e.matmul(pm[:, :], lhsT=C(C_BLK64), rhs=sqb[:], start=True, stop=True), reads=["cst", "sqb"], writes=[pmn])
                    S.op("dve", lambda e: e.tensor_scalar(out=rb[:], in0=pm[:, :], scalar1=EPS, scalar2=None, op0=ALU.add), reads=[pmn], writes=["rb"])
                    S.op("act", lambda e: e.activation(out=rb[:], in_=rb[:], func=AF.Sqrt), reads=["rb"], writes=["rb"])
                    S.op("dve", lambda e: e.reciprocal(out=rb[:], in_=rb[:]), reads=["rb"], writes=["rb"])
                    if which == 0:
                        S.op("dve", lambda e: e.scalar_tensor_tensor(out=qT[:].rearrange("p a b -> p (a b)"), in0=pb[:, :], scalar=gq, in1=rb[:],
                                                                     op0=ALU.mult, op1=ALU.mult), reads=[pbn, "rb", "gq"], writes=["qT"])
                    else:
                        for p4 in range(4):
                            S.op("dve", lambda e: e.scalar_tensor_tensor(out=kT_ring[:, p4, slot * 128:(slot + 1) * 128], in0=pb[:, p4 * 128:(p4 + 1) * 128],
                                                                         scalar=qkgain[:, 1:2], in1=rb[:, p4 * 128:(p4 + 1) * 128], op0=ALU.mult, op1=ALU.mult),
                                 reads=[pbn, "rb", "sm_qkgain"], writes=["kT_ring"])
                chk(2)
                pb, pbn = bank()
                proj_tm(pb, pbn, 1024, 512)
                S.op("act", lambda e: e.activation(out=V_ring[:, slot, :, 0:64], in_=pb[:, :].rearrange("p (a b) -> p a b", b=64), func=AF.Copy),
                     reads=[pbn], writes=["V_ring"])
                if it == 0:
                    S.op("pool", lambda e: e.memset(craw[:, :, 0:3], 0.0), writes=["craw"])
                else:
                    S.op("pool", lambda e: e.tensor_copy(out=craw[:, :, 0:3], in_=craw[:, :, 128:131]), reads=["craw"], writes=["craw"])
                for g3 in range(3):
                    pb, pbn = bank()
                    proj_fm(pb, pbn, 1536 + g3 * 512, 4)
                    S.op("act", lambda e: e.activation(out=craw[:, g3 * 4:(g3 + 1) * 4, 3:131], in_=pb[:, :].rearrange("p (a b) -> p a b", b=128), func=AF.Copy),
                         reads=[pbn], writes=["craw"])
                zs = sb(em, "zs", [128, 512])
                pb, pbn = bank()
                proj_tm(pb, pbn, 3072, 512)
                S.op("act", lambda e: e.activation(out=zs[:], in_=pb[:, :], func=AF.Silu), reads=[pbn], writes=["zs"])
                pb, pbn = bank()
                proj_tm(pb, pbn, 3584, 8)
                bet = st[:, 4:8]; gg = st[:, 8:12]
                S.op("act", lambda e: e.activation(out=bet, in_=pb[:, 0:4], func=AF.Sigmoid), reads=[pbn], writes=["bet"])
                S.op("dve", lambda e: e.tensor_tensor(out=gg, in0=pb[:, 4:8], in1=dtb, op=ALU.add), reads=[pbn, "sm_dtb"], writes=["gg"])
                S.op("act", lambda e: e.activation(out=gg, in_=gg, func=AF.Exp), reads=["gg"], writes=["gg"])
                S.op("act", lambda e: e.activation(out=gg, in_=gg, func=AF.Ln, bias=1.0), reads=["gg"], writes=["gg"])
                S.op("dve", lambda e: e.tensor_tensor(out=gg, in0=gg, in1=nexpA, op=ALU.mult), reads=["gg", "nexpA"], writes=["gg"])

                chk(3)
                Efs = [sb(em, "Ef%d" % i, [128, 256]) for i in range(2)]
                Eb = [sb(em, "Eb%d" % i, [128, 640], BF16) for i in range(2)]
                deltas = [d for d in range(5) if it - d >= 0]
                def attn_head(h):
                    p4, hf = h // 2, h % 2
                    pr = slice(64 * hf, 64 * hf + 64)
                    E = Eb[h % 2]; En = "Eb%d" % (h % 2)
                    Ef = Efs[h % 2]; Efn = "Ef%d" % (h % 2)
                    sa, san = bank()
                    sbk, sbn = bank()
                    for d in deltas:
                        sj = (it - d) % 5
                        dst = sa[:, d * 128:(d + 1) * 128] if d < 2 else sbk[:, (d - 2) * 128:(d - 1) * 128]
                        S.op("pe", lambda e: e.matmul(dst, lhsT=kT_ring[pr, p4, sj * 128:(sj + 1) * 128], rhs=qT[pr, p4, :], start=True, stop=True),
                             reads=["kT_ring", "qT"], writes=[san if d < 2 else sbn])
                    n01 = min(2, len(deltas))
                    S.op("act", lambda e: e.activation(out=Ef[:, 0:n01 * 128], in_=sa[:, 0:n01 * 128], func=AF.Exp), reads=[san], writes=[Efn])
                    S.op("dve", lambda e: e.tensor_tensor(out=E[:, 0:n01 * 128], in0=Ef[:, 0:n01 * 128],
                                                          in1=expB[:, h, 0:n01, :].rearrange("p a b -> p (a b)"), op=ALU.mult),
                         reads=[Efn, "expB"], writes=[En])
                    n2 = len(deltas) - 2
                    if n2 > 0:
                        S.op("act", lambda e: e.activation(out=E[:, 256:256 + n2 * 128], in_=sbk[:, 0:n2 * 128], func=AF.Exp, bias=cbias[:, h:h + 1]),
                             reads=[sbn, "sm_cbias"], writes=[En])
                        if n2 == 3:
                            S.op("pool", lambda e: e.memset(E[0:64, 512 + 64:640], 0.0), reads=[En], writes=[En])
                    yield
                    po = PB[4 + h // 4]; pon = "pb%d" % (4 + h // 4)
                    oc = (h % 4) * 65
                    for di, d in enumerate(deltas):
                        sj = (it - d) % 5
                        S.op("pe", lambda e: e.matmul(po[:, oc:oc + 65], lhsT=E[:, d * 128:(d + 1) * 128], rhs=V_ring[:, sj, h, :],
                                                      start=(di == 0), stop=(di == len(deltas) - 1)), reads=[En, "V_ring"], writes=[pon])
                    S.op("dve", lambda e: e.reciprocal(out=st[:, 16 + h:17 + h], in_=po[:, oc + 64:oc + 65]), reads=[pon], writes=["rinv%d" % h])
                    S.op("act", lambda e: e.activation(out=o_cat[:, h * 64:(h + 1) * 64], in_=po[:, oc:oc + 64], func=AF.Copy, scale=st[:, 16 + h:17 + h]),
                         reads=[pon, "rinv%d" % h], writes=["o_a%d" % h])
                for hp in range(0, 8, 2):
                    gens = [attn_head(hp + w_) for w_ in range(2)]
                    while gens:
                        for g_ in list(gens):
                            try:
                                next(g_)
                            except StopIteration:
                                gens.remove(g_)
                OA = ["o_a%d" % h for h in range(8)]

                chk(4)
                cs = sb(em, "cs", [128, 12, 128])
                acc = sb(em, "acc", [128, 128])
                for cc in range(12):
                    w0 = off["convw"] + cc * 4
                    S.op("dve", lambda e: e.tensor_scalar(out=acc[:], in0=craw[:, cc, 0:128], scalar1=small[:, w0:w0 + 1], scalar2=None, op0=ALU.mult),
                         reads=["craw", "sm_convw"], writes=["acc"])
                    for i3 in range(1, 4):
                        dst = cs[:, cc, :] if i3 == 3 else acc[:]
                        S.op("dve", lambda e: e.scalar_tensor_tensor(out=dst, in0=craw[:, cc, i3:i3 + 128], scalar=small[:, w0 + i3:w0 + i3 + 1], in1=acc[:],
                                                                     op0=ALU.mult, op1=ALU.add), reads=["craw", "sm_convw", "acc"],
                             writes=["cs"] if i3 == 3 else ["acc"])
                S.op("act", lambda e: e.activation(out=cs[:].rearrange("p a b -> p (a b)"), in_=cs[:].rearrange("p a b -> p (a b)"), func=AF.Silu),
                     reads=["cs"], writes=["cs"])
                for qk in range(2):
                    v4 = cs[:, qk * 4:(qk + 1) * 4, :].rearrange("p a b -> p (a b)")
                    S.op("act", lambda e: e.activation(out=sqb[:], in_=v4, func=AF.Square), reads=["cs"], writes=["sqb"])
                    pm, pmn = bank()
                    S.op("pe", lambda e: e.matmul(pm[:, :], lhsT=C(C_ONES), rhs=sqb[:], start=True, stop=True), reads=["cst", "sqb"], writes=[pmn])
                    S.op("dve", lambda e: e.tensor_scalar(out=rb[:], in0=pm[:, :], scalar1=EPS, scalar2=None, op0=ALU.add), reads=[pmn], writes=["rb"])
                    S.op("act", lambda e: e.activation(out=rb[:], in_=rb[:], func=AF.Sqrt), reads=["rb"], writes=["rb"])
                    S.op("dve", lambda e: e.reciprocal(out=rb[:], in_=rb[:]), reads=["rb"], writes=["rb"])
                    S.op("dve", lambda e: e.tensor_tensor(out=v4, in0=v4, in1=rb[:], op=ALU.mult), reads=["cs", "rb"], writes=["cs"])
                gcs = st[:, 24:28]; egq = st[:, 28:32]; egc = st[:, 32:36]; edec = st[:, 36:40]; egl = st[:, 40:48]; bge = st[:, 48:52]
                pq, pqn = quarter()
                S.op("pe", lambda e: e.matmul(pq[:, 0:4], lhsT=C(C_TRIT), rhs=gg, start=True, stop=True), reads=["cst", "gg"], writes=[pqn])
                S.op("dve", lambda e: e.tensor_copy(out=gcs, in_=pq[:, 0:4]), reads=[pqn], writes=["gcs"])
                S.op("act", lambda e: e.activation(out=egc, in_=gcs, func=AF.Exp), reads=["gcs"], writes=["egc"])
                S.op("dve", lambda e: e.tensor_scalar(out=egq, in0=egc, scalar1=128.0 ** -0.5, scalar2=None, op0=ALU.mult), reads=["egc"], writes=["egq"])
                S.op("dve", lambda e: e.tensor_tensor(out=bge, in0=egc, in1=bet, op=ALU.mult), reads=["egc", "bet"], writes=["bge"])
                pq, pqn = quarter()
                S.op("pe", lambda e: e.matmul(pq[:, 0:4], lhsT=C(C_SELT), rhs=gcs, start=True, stop=True), reads=["cst", "gcs"], writes=[pqn])
                S.op("pe", lambda e: e.matmul(pq[:, 4:8], lhsT=C(C_SEL0), rhs=gcs, start=True, stop=True), reads=["cst", "gcs"], writes=[pqn])
                S.op("pe", lambda e: e.matmul(pq[:, 8:12], lhsT=C(C_SEL1), rhs=gcs, start=True, stop=True), reads=["cst", "gcs"], writes=[pqn])
                S.op("dve", lambda e: e.tensor_tensor(out=edec, in0=pq[:, 0:4], in1=gcs, op=ALU.subtract), reads=[pqn, "gcs"], writes=["edec"])
                S.op("act", lambda e: e.activation(out=edec, in_=edec, func=AF.Exp), reads=["edec"], writes=["edec"])
                S.op("act", lambda e: e.activation(out=egl, in_=pq[:, 4:12], func=AF.Exp), reads=[pqn], writes=["egl"])

                chk(5)
                Gs = [{n: sb(em, "g" + str(w_) + "_" + n, [128, 128]) for n in ['gb', 'Dm', 'Ds', 'L', 'U', 'P0', 'P1', 'Lp', 'Lr0', 'Lr1', 'Ur0', 'Ur1', 'ktok', 'kbg', 'kdec', 'vbeta', 'wT', 'u', 'vnew', 'intra', 'intraT', 't1']} for w_ in range(2)]
                for w_ in range(2):
                    S.op("pool", lambda e: e.memset(Gs[w_]["vnew"][:], 0.0), writes=["vnew_" + str(w_)])
                GSET = set(['gb', 'Dm', 'Ds', 'L', 'U', 'P0', 'P1', 'Lp', 'Lr0', 'Lr1', 'Ur0', 'Ur1', 'ktok', 'kbg', 'kdec', 'vbeta', 'wT', 'u', 'vnew', 'intra', 'intraT', 't1'])

                def gdn_head(h, G, sfx):
                    def gop(e_, fn_, reads=(), writes=()):
                        def mp(n_):
                            if n_ in GSET:
                                return n_ + sfx
                            if n_ == "Sst":
                                return "Sst" + str(h)
                            return n_
                        return S.op(e_, fn_, reads=[mp(n_) for n_ in reads], writes=[mp(n_) for n_ in writes])
                    qTh = cs[:, h, :]; kTh = cs[:, 4 + h, :]; vTh = cs[:, 8 + h, :]
                    gop("dve", lambda e: e.tensor_copy(out=G["gb"][:], in_=cv(st[:, 8 + h:9 + h], [[0, 128]])), reads=["gg"], writes=["gb"])
                    yield
                    pg, pgn = quarter()
                    gop("pe", lambda e: e.matmul(pg, lhsT=G["gb"][:], rhs=C(C_TRIT), start=True, stop=True), reads=["gb", "cst"], writes=[pgn])
                    gop("dve", lambda e: e.tensor_scalar(out=G["Dm"][:], in0=pg, scalar1=gcs[:, h:h + 1], scalar2=0.0, op0=ALU.subtract, op1=ALU.max),
                         reads=[pgn, "gcs"], writes=["Dm"])
                    yield
                    gop("act", lambda e: e.activation(out=G["Dm"][:], in_=G["Dm"][:], func=AF.Exp, scale=-1.0), reads=["Dm"], writes=["Dm"])
                    yield
                    gop("dve", lambda e: e.tensor_tensor(out=G["Ds"][:], in0=G["Dm"][:], in1=C(C_STRI), op=ALU.mult), reads=["Dm", "cst"], writes=["Ds"])
                    yield
                    gop("dve", lambda e: e.tensor_tensor(out=G["Dm"][:], in0=G["Dm"][:], in1=C(C_TRI), op=ALU.mult), reads=["Dm", "cst"], writes=["Dm"])
                    yield
                    pk, pkn = quarter()
                    gop("act", lambda e: e.activation(out=G["gb"][:], in_=kTh, func=AF.Copy), reads=["cs"], writes=["gb"])
                    yield
                    gop("pe", lambda e: e.matmul(pk, lhsT=kTh, rhs=G["gb"][:], start=True, stop=True), reads=["cs", "gb"], writes=[pkn])
                    gop("dve", lambda e: e.scalar_tensor_tensor(out=G["L"][:], in0=pk, scalar=bet[:, h:h + 1], in1=G["Ds"][:], op0=ALU.mult, op1=ALU.mult),
                         reads=[pkn, "bet", "Ds"], writes=["L"])
                    yield
                    pqk, pqkn = quarter()
                    gop("pe", lambda e: e.matmul(pqk, lhsT=qTh, rhs=kTh, start=True, stop=True), reads=["cs"], writes=[pqkn])
                    gop("dve", lambda e: e.scalar_tensor_tensor(out=G["intra"][:], in0=pqk, scalar=128.0 ** -0.5, in1=G["Dm"][:], op0=ALU.mult, op1=ALU.mult),
                         reads=[pqkn, "Dm"], writes=["intra"])
                    yield
                    pt, ptn = quarter()
                    gop("pe", lambda e: e.transpose(out=pt, in_=G["L"][:], identity=C(C_IDENT)), reads=["L", "cst"], writes=[ptn])
                    gop("act", lambda e: e.activation(out=G["U"][:], in_=pt, func=AF.Copy), reads=[ptn], writes=["U"])
                    yield
                    gop("dve", lambda e: e.scalar_tensor_tensor(out=G["P0"][:], in0=pt, scalar=-1.0, in1=C(C_IDENT), op0=ALU.mult, op1=ALU.add), reads=[ptn, "cst", "U"], writes=["P0"])
                    yield
                    pt2, pt2n = quarter()
                    gop("pe", lambda e: e.transpose(out=pt2, in_=G["intra"][:], identity=C(C_IDENT)), reads=["intra", "cst"], writes=[pt2n])
                    gop("act", lambda e: e.activation(out=G["intraT"][:], in_=pt2, func=AF.Copy), reads=[pt2n], writes=["intraT"])
                    yield
                    pt3, pt3n = quarter()
                    gop("pe", lambda e: e.transpose(out=pt3, in_=kTh, identity=C(C_IDENT)), reads=["cs", "cst"], writes=[pt3n])
                    gop("dve", lambda e: e.tensor_scalar(out=G["kbg"][:], in0=pt3, scalar1=bge[:, h:h + 1], scalar2=None, op0=ALU.mult),
                         reads=[pt3n, "bge"], writes=["kbg"])
                    yield
                    gop("act", lambda e: e.activation(out=G["ktok"][:], in_=pt3, func=AF.Copy), reads=[pt3n], writes=["ktok"])
                    yield
                    pt4, pt4n = quarter()
                    gop("pe", lambda e: e.transpose(out=pt4, in_=vTh, identity=C(C_IDENT)), reads=["cs", "cst"], writes=[pt4n])
                    gop("act", lambda e: e.activation(out=G["vbeta"][:], in_=pt4, func=AF.Copy, scale=bet[:, h:h + 1]), reads=[pt4n, "bet"], writes=["vbeta"])
                    yield
                    Lc, Lcn, Uc, Ucn, Pc, Pcn = G["L"], "L", G["U"], "U", G["P0"], "P0"
                    for kk in range(1, 6):
                        pl, pln = quarter()
                        gop("pe", lambda e: e.matmul(pl, lhsT=Uc[:], rhs=Lc[:], start=True, stop=True), reads=[Ucn, Lcn], writes=[pln])
                        gop("dve", lambda e: e.tensor_tensor(out=G["Lp"][:], in0=pl, in1=C(C_IDENT), op=ALU.add), reads=[pln, "cst"], writes=["Lp"])
                        yield
                        if kk < 5:
                            pu, pun = quarter()
                            gop("pe", lambda e: e.matmul(pu, lhsT=Lc[:], rhs=Uc[:], start=True, stop=True), reads=[Ucn, Lcn], writes=[pun])
                            Ln_, Un_ = "Lr%d" % (kk % 2), "Ur%d" % (kk % 2)
                            gop("act", lambda e: e.activation(out=G[Ln_][:], in_=pl, func=AF.Copy), reads=[pln], writes=[Ln_])
                            yield
                            gop("act", lambda e: e.activation(out=G[Un_][:], in_=pu, func=AF.Copy), reads=[pun], writes=[Un_])
                            yield
                        pp, ppn = quarter()
                        gop("pe", lambda e: e.matmul(pp, lhsT=G["Lp"][:], rhs=Pc[:], start=True, stop=True), reads=["Lp", Pcn], writes=[ppn])
                        Pn_ = "P%d" % (kk % 2)
                        gop("dve", lambda e: e.tensor_copy(out=G[Pn_][:], in_=pp), reads=[ppn], writes=[Pn_])
                        yield
                        Pc, Pcn = G[Pn_], Pn_
                        if kk < 5:
                            Lc, Lcn, Uc, Ucn = G[Ln_], Ln_, G[Un_], Un_
                    pw, pwn = quarter()
                    gop("pe", lambda e: e.matmul(pw, lhsT=G["kbg"][:], rhs=Pc[:], start=True, stop=True), reads=["kbg", Pcn], writes=[pwn])
                    gop("act", lambda e: e.activation(out=G["wT"][:], in_=pw, func=AF.Copy), reads=[pwn], writes=["wT"])
                    yield
                    pu2, pu2n = quarter()
                    gop("pe", lambda e: e.matmul(pu2, lhsT=Pc[:], rhs=G["vbeta"][:], start=True, stop=True), reads=["vbeta", Pcn], writes=[pu2n])
                    gop("dve", lambda e: e.tensor_copy(out=G["u"][:], in_=pu2), reads=[pu2n], writes=["u"])
                    yield
                    Sh = Sst[:, h, :]
                    for c in range(2):
                        r = slice(64 * c, 64 * c + 64)
                        gop("dve", lambda e: e.tensor_scalar(out=G["kdec"][:], in0=G["ktok"][:], scalar1=edec[:, h:h + 1], scalar2=cst[:, C_TRI, 64 * c:64 * c + 1],
                                                              op0=ALU.mult, op1=ALU.mult), reads=["ktok", "edec", "cst"], writes=["kdec"])
                        yield
                        pa, pan = quarter()
                        gop("pe", lambda e: e.matmul(pa, lhsT=G["wT"][:], rhs=Sh, start=True, stop=True), reads=["wT", "Sst"], writes=[pan])
                        gop("dve", lambda e: e.scalar_tensor_tensor(out=G["vnew"][r, :], in0=pa[r, :], scalar=-1.0, in1=G["u"][r, :], op0=ALU.mult, op1=ALU.add),
                             reads=["u", pan], writes=["vnew"])
                        yield
                        po1, po1n = quarter()
                        gop("pe", lambda e: e.matmul(po1, lhsT=cs[:, h, :], rhs=Sh, start=True, stop=True), reads=["cs", "Sst"], writes=[po1n])
                        gop("act", lambda e: e.activation(out=G["t1"][r, :], in_=po1[r, :], func=AF.Copy, scale=egq[r, h:h + 1]), reads=[po1n, "egq"], writes=["t1"])
                        yield
                        po2, po2n = quarter()
                        gop("pe", lambda e: e.matmul(po2, lhsT=G["intraT"][:], rhs=G["vnew"][:], start=True, stop=True),
                             reads=["intraT", "vnew"], writes=[po2n])
                        gop("dve", lambda e: e.tensor_tensor(out=o_cat[r, 512 + h * 128:512 + (h + 1) * 128], in0=po2[r, :], in1=G["t1"][r, :], op=ALU.add),
                             reads=["t1", po2n], writes=["o_b%d" % h])
                        yield
                        psn, psnn = quarter()
                        gop("pe", lambda e: e.matmul(psn, lhsT=G["kdec"][:], rhs=G["vnew"][:], start=True, stop=True), reads=["kdec", "vnew"], writes=[psnn])
                        gop("dve", lambda e: e.scalar_tensor_tensor(out=Sh, in0=Sh, scalar=egl[:, c * 4 + h:c * 4 + h + 1], in1=psn, op0=ALU.mult, op1=ALU.add),
                             reads=["Sst", "egl", psnn], writes=["Sst"])
                        yield

                for hp in range(0, 4, 2):
                    gens = [gdn_head(hp + w_, Gs[w_], "_" + str(w_)) for w_ in range(2)]
                    while gens:
                        for g_ in list(gens):
                            try:
                                next(g_)
                            except StopIteration:
                                gens.remove(g_)
                OB = ["o_b%d" % h for h in range(4)]

                chk(6)
                catb = sb(em, "catb", [128, 1024], BF16)
                S.op("act", lambda e: e.activation(out=xn[:, 0:512], in_=o_cat[:, 0:512], func=AF.Square, accum_out=st[:, 52:53]), reads=OA, writes=["xn", "ssa"])
                S.op("dve", lambda e: e.tensor_scalar(out=st[:, 52:53], in0=st[:, 52:53], scalar1=1.0 / 512, scalar2=EPS, op0=ALU.mult, op1=ALU.add),
                     reads=["ssa"], writes=["ssa"])
                S.op("act", lambda e: e.activation(out=st[:, 52:53], in_=st[:, 52:53], func=AF.Sqrt), reads=["ssa"], writes=["ssa"])
                S.op("dve", lambda e: e.reciprocal(out=st[:, 52:53], in_=st[:, 52:53]), reads=["ssa"], writes=["ssa"])
                S.op("dve", lambda e: e.scalar_tensor_tensor(out=catb[:, 0:512], in0=o_cat[:, 0:512], scalar=st[:, 52:53], in1=aog, op0=ALU.mult, op1=ALU.mult),
                     reads=OA + ["ssa", "sm_aog"], writes=["catb"])
                for h in range(4):
                    sl = slice(512 + h * 128, 512 + (h + 1) * 128)
                    S.op("act", lambda e: e.activation(out=xn[:, 0:128], in_=o_cat[:, sl], func=AF.Square, accum_out=st[:, 56 + h:57 + h]),
                         reads=OB, writes=["xn", "ssb%d" % h])
                S.op("dve", lambda e: e.tensor_scalar(out=st[:, 56:60], in0=st[:, 56:60], scalar1=1.0 / 128, scalar2=EPS, op0=ALU.mult, op1=ALU.add),
                     reads=["ssb%d" % h for h in range(4)], writes=["ssb"])
                S.op("act", lambda e: e.activation(out=st[:, 56:60], in_=st[:, 56:60], func=AF.Sqrt), reads=["ssb"], writes=["ssb"])
                S.op("dve", lambda e: e.reciprocal(out=st[:, 56:60], in_=st[:, 56:60]), reads=["ssb"], writes=["ssb"])
                for h in range(4):
                    sl = slice(512 + h * 128, 512 + (h + 1) * 128)
                    S.op("dve", lambda e: e.scalar_tensor_tensor(out=xn[:, 0:128], in0=o_cat[:, sl], scalar=st[:, 56 + h:57 + h], in1=dog, op0=ALU.mult, op1=ALU.mult),
                         reads=OB + ["ssb", "sm_dog"], writes=["xn"])
                    S.op("dve", lambda e: e.tensor_tensor(out=catb[:, sl], in0=xn[:, 0:128], in1=zs[:, h * 128:(h + 1) * 128], op=ALU.mult),
                         reads=["xn", "zs"], writes=["catb"])
                cTb = sb(em, "cTb", [128, 8, 128], BF16)
                pb, pbn = bank()
                pbb = pb[:, :].bitcast(BF16)
                for k in range(8):
                    S.op("pe", lambda e: e.transpose(out=pbb[:, k * 128:(k + 1) * 128], in_=catb[:, k * 128:(k + 1) * 128], identity=identb[:]),
                         reads=["catb", "identb"], writes=[pbn])
                S.op("act", lambda e: e.activation(out=cTb[:].rearrange("p a b -> p (a b)"), in_=pbb, func=AF.Copy), reads=[pbn], writes=["cTb"])
                for nh in range(2):
                    pb, pbn = bank()
                    for k in range(8):
                        S.op("pe", lambda e: e.matmul(pb[:, :], lhsT=cTb[:, k, :], rhs=w_out_sb[:, k, nh * 512:(nh + 1) * 512], start=(k == 0), stop=(k == 7)),
                             reads=["cTb"] + W_OUT, writes=[pbn])
                    S.op("dve", lambda e: e.tensor_tensor(out=xn[:, nh * 512:(nh + 1) * 512], in0=pb[:, :], in1=gate_rows[:, 0, nh * 512:(nh + 1) * 512], op=ALU.mult),
                         reads=[pbn, "gate_rows"], writes=["xn"])
                    S.op("dve", lambda e: e.tensor_tensor(out=x_sb[:, nh * 512:(nh + 1) * 512], in0=x_sb[:, nh * 512:(nh + 1) * 512], in1=xn[:, nh * 512:(nh + 1) * 512], op=ALU.add),
                         reads=["x", "xn"], writes=["x"])
                if dbg:
                    S.dma("sp", lambda e: e.dma_start(out=dbg_o[ti * 128:(ti + 1) * 128, :], in_=x_sb[:]), reads=["x"])
                S.barrier()
              with ExitStack() as em:
                  try:
                      mixer(em)
                  except _Stop:
                      S.barrier()

            if do_peer:
                with ExitStack() as ep:
                    xn = sb(ep, "xn2", [128, 1024])
                    junk = sb(ep, "junk2", [128, 1024])
                    st = sb(ep, "st2", [128, 64])
                    h2Tb = sb(ep, "h2Tb", [128, 8, 128], BF16)
                    h2 = sb(ep, "h2", [128, 1024], BF16)
                    S.op("act", lambda e: e.activation(out=junk[:], in_=x_sb[:], func=AF.Square, accum_out=st[:, 0:1]), reads=["x"], writes=["junk", "ss"])
                    S.op("dve", lambda e: e.tensor_scalar(out=st[:, 1:2], in0=st[:, 0:1], scalar1=1.0 / 1024, scalar2=EPS, op0=ALU.mult, op1=ALU.add),
                         reads=["ss"], writes=["rs"])
                    S.op("act", lambda e: e.activation(out=st[:, 1:2], in_=st[:, 1:2], func=AF.Sqrt), reads=["rs"], writes=["rs"])
                    S.op("dve", lambda e: e.reciprocal(out=st[:, 1:2], in_=st[:, 1:2]), reads=["rs"], writes=["rs"])
                    S.op("act", lambda e: e.activation(out=xn[:], in_=x_sb[:], func=AF.Copy, scale=st[:, 1:2]), reads=["x", "rs"], writes=["xn"])
                    for half in range(2):
                        pb, pbn = bank()
                        for kk in range(4):
                            k = half * 4 + kk
                            S.op("pe", lambda e: e.transpose(out=pb[:, kk * 128:(kk + 1) * 128], in_=xn[:, k * 128:(k + 1) * 128], identity=C(C_IDENT)),
                                 reads=["xn", "cst"], writes=[pbn])
                        for kk in range(4):
                            k = half * 4 + kk
                            a_ap = small[:, off["A2"] + k * 2 + b:off["A2"] + k * 2 + b + 1]
                            s_ap = modv(24 + k, b)
                            S.op("dve", lambda e: e.tensor_scalar(out=h2Tb[:, k, :], in0=pb[:, kk * 128:(kk + 1) * 128], scalar1=a_ap, scalar2=s_ap,
                                                                  op0=ALU.mult, op1=ALU.add), reads=[pbn, "A2", "modT"], writes=["h2Tb"])
                    pb, pbn = bank()
                    pbb = pb[:, :].bitcast(BF16)
                    for k in range(8):
                        S.op("pe", lambda e: e.transpose(out=pbb[:, k * 128:(k + 1) * 128], in_=h2Tb[:, k, :], identity=identb[:]),
                             reads=["h2Tb", "identb"], writes=[pbn])
                    S.op("act", lambda e: e.activation(out=h2[:], in_=pbb, func=AF.Copy), reads=[pbn], writes=["h2"])
                    qn = sb(ep, "qn", [128, 2048], BF16)
                    qbanks = []
                    QSS = ["qss%d" % h for h in range(8)]
                    for nb in range(4):
                        pb, pbn = bank()
                        qbanks.append((pb, pbn))
                        for k in range(8):
                            S.op("pe", lambda e: e.matmul(pb[:, :], lhsT=h2Tb[:, k, :], rhs=wq_sb[:, k, nb * 512:(nb + 1) * 512], start=(k == 0), stop=(k == 7)),
                                 reads=["h2Tb"] + W_Q, writes=[pbn])
                        for hh in range(2):
                            hq = nb * 2 + hh
                            jr = (hq % 4) * 256
                            S.op("act", lambda e: e.activation(out=junk[:, jr:jr + 256], in_=pb[:, hh * 256:(hh + 1) * 256], func=AF.Square, accum_out=st[:, 8 + hq:9 + hq]),
                                 reads=[pbn], writes=["junk%d" % (hq % 4), "qss%d" % hq])
                    S.op("dve", lambda e: e.tensor_scalar(out=st[:, 8:16], in0=st[:, 8:16], scalar1=1.0 / 256, scalar2=EPS, op0=ALU.mult, op1=ALU.add),
                         reads=QSS, writes=["qssall"] + QSS)
                    S.op("act", lambda e: e.activation(out=st[:, 8:16], in_=st[:, 8:16], func=AF.Sqrt), reads=["qssall"], writes=["qssall"])
                    S.op("dve", lambda e: e.reciprocal(out=st[:, 8:16], in_=st[:, 8:16]), reads=["qssall"], writes=["qssall"])
                    for nb in range(4):
                        pb, pbn = qbanks[nb]
                        for hh in range(2):
                            hq = nb * 2 + hh
                            S.op("dve", lambda e: e.scalar_tensor_tensor(out=qn[:, hq * 256:(hq + 1) * 256], in0=pb[:, hh * 256:(hh + 1) * 256], scalar=st[:, 8 + hq:9 + hq],
                                                                         in1=qg, op0=ALU.mult, op1=ALU.mult), reads=[pbn, "qssall", "sm_qg"], writes=["qn%d" % hq])
                    QN = ["qn%d" % h for h in range(8)]
                    qnT = sb(ep, "qnT", [128, 16, 128], BF16)
                    for half in range(2):
                        pb, pbn = bank()
                        pbb = pb[:, :].bitcast(BF16)
                        for kk in range(8):
                            k = half * 8 + kk
                            S.op("pe", lambda e: e.transpose(out=pbb[:, kk * 128:(kk + 1) * 128], in_=qn[:, k * 128:(k + 1) * 128], identity=identb[:]),
                                 reads=QN + ["identb"], writes=[pbn])
                        S.op("act", lambda e: e.activation(out=qnT[:, half * 8:(half + 1) * 8, :].rearrange("p a b -> p (a b)"), in_=pbb, func=AF.Copy),
                             reads=[pbn], writes=["qnT"])
                    W = 2
                    ssc = [sb(ep, "ssc%d" % i, [128, 256]) for i in range(W)]
                    ssr = [sb(ep, "ssr%d" % i, [128, 128]) for i in range(W)]
                    mx = [sb(ep, "mx%d" % i, [128, 32]) for i in range(W)]
                    mi = [sb(ep, "mi%d" % i, [128, 32], U32) for i in range(W)]
                    cand_s = [sb(ep, "cand_s%d" % i, [128, 256]) for i in range(W)]
                    cand_r = [sb(ep, "cand_r%d" % i, [128, 256]) for i in range(W)]
                    mif = sb(ep, "mif", [128, 8, 32])
                    ts = sb(ep, "ts", [128, 8, 16])
                    posa = sb(ep, "posa", [128, 8, 16], U32)
                    pab = sb(ep, "pab", [128, 2, 128], U32)
                    pabf = sb(ep, "pabf", [128, 2, 128])
                    isel = sb(ep, "isel", [128, 2, 128])
                    eid = sb(ep, "eid", [128, 128])
                    eidi = sb(ep, "eidi", [128, 128], I32)

                    def head_chain(hq, w):
                        sc = ssc[w]; scn = "ssc%d" % w
                        pb, pbn = bank()
                        for hf in range(2):
                            S.op("pe", lambda e: e.matmul(pb[:, hf * 128:(hf + 1) * 128], lhsT=qnT[:, hq * 2 + hf, :], rhs=skT[:, hf, :], start=True, stop=True),
                                 reads=["qnT", "sk1", "sk2"], writes=[pbn])
                        S.op("act", lambda e: e.activation(out=sc[:], in_=pb[:, 0:256], func=AF.Copy), reads=[pbn], writes=[scn])
                        yield
                        mxn, min_, srn = "mx%d" % w, "mi%d" % w, "ssr%d" % w
                        for hf in range(2):
                            s_ = sc[:, hf * 128:(hf + 1) * 128]
                            m0 = mx[w][:, hf * 16:hf * 16 + 8]; m1 = mx[w][:, hf * 16 + 8:hf * 16 + 16]
                            S.op("dve", lambda e: e.max(out=m0, in_=s_), reads=[scn], writes=[mxn]); yield
                            S.op("dve", lambda e: e.max_index(out=mi[w][:, hf * 16:hf * 16 + 8], in_max=m0, in_values=s_), reads=[scn, mxn], writes=[min_]); yield
                            S.op("dve", lambda e: e.match_replace(out=ssr[w][:], in_to_replace=m0, in_values=s_, imm_value=-1e30), reads=[scn, mxn], writes=[srn]); yield
                            S.op("dve", lambda e: e.max(out=m1, in_=ssr[w][:]), reads=[srn], writes=[mxn]); yield
                            S.op("dve", lambda e: e.max_index(out=mi[w][:, hf * 16 + 8:hf * 16 + 16], in_max=m1, in_values=ssr[w][:]), reads=[srn, mxn], writes=[min_]); yield
                        S.op("dve", lambda e: e.tensor_copy(out=mif[:, hq, :], in_=mi[w][:]), reads=[min_], writes=["mif%d" % hq]); yield
                        csn, crn = "cand_s%d" % w, "cand_r%d" % w
                        S.op("dve", lambda e: e.tensor_tensor(out=cand_s[w][:].rearrange("p (a b) -> p a b", b=16), in0=cv(mx[w][:, 0:1], [[1, 16], [0, 16]]),
                                                              in1=cv(mx[w][:, 16:17], [[0, 16], [1, 16]]), op=ALU.add), reads=[mxn], writes=[csn]); yield
                        S.op("dve", lambda e: e.max(out=ts[:, hq, 0:8], in_=cand_s[w][:]), reads=[csn], writes=["ts%d" % hq]); yield
                        S.op("dve", lambda e: e.match_replace(out=cand_r[w][:], in_to_replace=ts[:, hq, 0:8], in_values=cand_s[w][:], imm_value=-1e30),
                             reads=[csn, "ts%d" % hq], writes=[crn]); yield
                        S.op("dve", lambda e: e.max(out=ts[:, hq, 8:16], in_=cand_r[w][:]), reads=[crn], writes=["ts%d" % hq]); yield
                        S.op("dve", lambda e: e.max_index(out=posa[:, hq, 0:8], in_max=ts[:, hq, 0:8], in_values=cand_s[w][:]), reads=[csn, "ts%d" % hq], writes=["pos%d" % hq]); yield
                        S.op("dve", lambda e: e.max_index(out=posa[:, hq, 8:16], in_max=ts[:, hq, 8:16], in_values=cand_r[w][:]), reads=[crn, "ts%d" % hq], writes=["pos%d" % hq]); yield

                    for h0 in range(0, 8, W):
                        gens = [head_chain(h0 + w, w) for w in range(W)]
                        while gens:
                            for g_ in list(gens):
                                try:
                                    next(g_)
                                except StopIteration:
                                    gens.remove(g_)
                    TS = ["ts%d" % h for h in range(8)]
                    POS = ["pos%d" % h for h in range(8)]
                    MIF = ["mif%d" % h for h in range(8)]
                    posf = posa[:].rearrange("p a b -> p (a b)")
                    S.op("dve", lambda e: e.tensor_single_scalar(out=pab[:, 0, :], in_=posf, scalar=4, op=ALU.logical_shift_right), reads=POS, writes=["pab0"])
                    S.op("dve", lambda e: e.tensor_single_scalar(out=pab[:, 1, :], in_=posf, scalar=15, op=ALU.bitwise_and), reads=POS, writes=["pab1"])
                    S.op("dve", lambda e: e.tensor_copy(out=pabf[:].rearrange("p a b -> p (a b)"), in_=pab[:].rearrange("p a b -> p (a b)")), reads=["pab0", "pab1"], writes=["pabf"])
                    for g4 in range(2):
                        for w_ in range(2):
                            scr = junk[:, (w_ * 1024) % 1024:(w_ * 1024) % 1024 + 1024].rearrange("p (h k a) -> p h k a", h=4, k=16)
                            scn_ = "junk"
                            S.op("dve", lambda e: e.tensor_tensor(out=scr, in0=cv(pabf[:, w_, g4 * 64:g4 * 64 + 1], [[16, 4], [1, 16], [0, 16]]),
                                                                  in1=cv(iota16[:, 0:1], [[0, 4], [0, 16], [1, 16]]), op=ALU.is_equal),
                                 reads=["pabf", "sm_iota16"], writes=["junk0", "junk1", "junk2", "junk3"])
                            S.op("dve", lambda e: e.tensor_tensor(out=scr, in0=scr, in1=cv(mif[:, g4 * 4, w_ * 16:w_ * 16 + 1], [[32, 4], [0, 16], [1, 16]]), op=ALU.mult),
                                 reads=["junk0"] + MIF, writes=["junk0", "junk1", "junk2", "junk3"])
                            S.op("dve", lambda e: e.tensor_reduce(out=isel[:, w_, g4 * 64:(g4 + 1) * 64].rearrange("p (h k) -> p h k", k=16), in_=scr, axis=AX.X, op=ALU.add),
                                 reads=["junk0"], writes=["isel%d%d" % (w_, g4)])
                    S.op("dve", lambda e: e.scalar_tensor_tensor(out=eid[:], in0=isel[:, 0, :], scalar=128.0, in1=isel[:, 1, :], op0=ALU.mult, op1=ALU.add),
                         reads=["isel00", "isel01", "isel10", "isel11"], writes=["eid"])
                    S.op("dve", lambda e: e.memset(junk[:, 0:8], 0.0), reads=["eid"], writes=["junk0", "eid"])
                    S.op("dve", lambda e: e.tensor_copy(out=eidi[:], in_=eid[:]), reads=["eid"], writes=["eidi"])
                    ge = sb(ep, "ge", [128, 8, 16])
                    S.op("dve", lambda e: e.tensor_scalar(out=st[:, 24:32], in0=cv(ts[:, 0, 0:1], [[16, 8]]), scalar1=-1.0, scalar2=None, op0=ALU.mult),
                         reads=TS, writes=["nts0"])
                    for hq in range(8):
                        S.op("act", lambda e: e.activation(out=ge[:, hq, :], in_=ts[:, hq, :], func=AF.Exp, bias=st[:, 24 + hq:25 + hq], accum_out=st[:, 32 + hq:33 + hq]),
                             reads=["ts%d" % hq, "nts0"], writes=["ge", "zs%d" % hq])
                    S.op("dve", lambda e: e.reciprocal(out=st[:, 32:40], in_=st[:, 32:40]), reads=["zs%d" % h for h in range(8)], writes=["rz"])
                    S.op("dve", lambda e: e.tensor_tensor(out=ge[:], in0=ge[:], in1=cv(st[:, 32:33], [[1, 8], [0, 16]]), op=ALU.mult), reads=["ge", "rz"], writes=["ge"])
                    NB = 4
                    uv = [sb(ep, "uv%d" % i, [128, 2048], BF16)[:] for i in range(NB)]
                    uvn = [["uv%d" % i] for i in range(NB)]
                    uv += [qn[:], qnT[:].rearrange("p a b -> p (a b)"), xn[:].bitcast(BF16)]
                    uvn += [QN, ["qnT"], ["xn"]]
                    NB = len(uv)
                    dg = [sb(ep, "dg%d" % i, [128, 128], BF16) for i in range(NB)]
                    pre = sb(ep, "pre", [128, 128])
                    gl = sb(ep, "gl", [128, 128])
                    wgt = sb(ep, "wgt", [128, 128])
                    S.op("dve", lambda e: e.memset(pre[:], 0.0), writes=["pre"])
                    gef = ge[:].rearrange("p a b -> p (a b)")
                    def consume(j):
                        u_ = uv[j % NB]; un = uvn[j % NB]
                        d_ = dg[j % NB]; dn = "dg%d" % (j % NB)
                        S.op("act", lambda e: e.activation(out=gl[:, j:j + 1], in_=pre[:, j:j + 1], func=AF.Gelu), reads=["pre%d" % j, "pre%d" % (j + 1)], writes=["gl%d" % j])
                        S.op("act", lambda e: e.activation(out=wgt[:, j:j + 1], in_=gl[:, j:j + 1], func=AF.Copy, scale=gef[:, j:j + 1]), reads=["gl%d" % j, "ge"], writes=["w%d" % j])
                        S.op("act", lambda e: e.activation(out=d_[:], in_=identb[:], func=AF.Copy, scale=wgt[:, j:j + 1]), reads=["identb", "w%d" % j], writes=[dn])
                        for nh in range(2):
                            S.op("pe", lambda e: e.matmul(PB[4 + nh][:, :], lhsT=d_[:], rhs=u_[:, 1024 + nh * 512:1024 + (nh + 1) * 512], start=(j == 0), stop=(j == 127)),
                                 reads=[dn] + un, writes=["pb%d" % (4 + nh)])

                    for j in range(128):
                        u_ = uv[j % NB]; un = uvn[j % NB]
                        S.dma("pool", lambda e: e.indirect_dma_start(out=u_, out_offset=None, in_=tab,
                                                                     in_offset=bass.IndirectOffsetOnAxis(ap=eidi[:, j:j + 1], axis=0)),
                              reads=["eidi"], writes=un)
                        S.op("dve", lambda e: e.scalar_tensor_tensor(out=junk[:], in0=u_[:, 0:1024], scalar=1.0, in1=h2[:], op0=ALU.mult, op1=ALU.mult,
                                                                     accum_out=pre[:, j:j + 1]), reads=un + ["h2"], writes=["junk", "junk0", "junk1", "junk2", "junk3", "pre%d" % j])
                        if j >= 1:
                            consume(j - 1)
                    S.op("dve", lambda e: e.memset(junk[:, 0:8], 0.0), writes=["junk", "junk0", "pre128"])
                    consume(127)
                    for nh in range(2):
                        S.op("dve", lambda e: e.tensor_tensor(out=junk[:, nh * 512:(nh + 1) * 512], in0=PB[4 + nh][:, :], in1=gate_rows[:, 1, nh * 512:(nh + 1) * 512], op=ALU.mult),
                             reads=["pb%d" % (4 + nh), "gate_rows"], writes=["junk", "junk0", "junk1", "junk2", "junk3"])
                        S.op("dve", lambda e: e.tensor_tensor(out=x_sb[:, nh * 512:(nh + 1) * 512], in0=x_sb[:, nh * 512:(nh + 1) * 512], in1=junk[:, nh * 512:(nh + 1) * 512], op=ALU.add),
                             reads=["x", "junk"], writes=["x"])
                    S.dma("sp", lambda e: e.dma_start(out=out[ti * 128:(ti + 1) * 128, :], in_=x_sb[:]), reads=["x"])
                    S.barrier()
            else:
                S.dma("sp", lambda e: e.dma_start(out=out[ti * 128:(ti + 1) * 128, :], in_=x_sb[:]), reads=["x"])
                S.barrier()
        S.barrier(engines=("sp",))
        print("instructions", S.n_inst, "waits", S.n_wait, S.cnt, S.dval)
    return nc


def prep_inputs(inp, core):
    f = lambda a: np.ascontiguousarray(np.asarray(a, dtype=np.float32))
    b0 = core * 2
    rep = lambda v: f(np.broadcast_to(np.asarray(v, np.float32).reshape(1, -1), (128, np.asarray(v).size)))
    m = {}
    m["x"] = f(inp["x"][b0:b0 + 2].reshape(4096, 1024))
    m["cT"] = f(np.asarray(inp["c"])[b0:b0 + 2].reshape(2, 8, 128).transpose(2, 1, 0))
    m["w_ada"] = f(inp["w_ada"][0])
    m["b_adaT"] = f(np.asarray(inp["b_ada"])[0].reshape(48, 128).T)
    ba = np.asarray(inp["b_ada"])[0]
    m["b_gate"] = f(np.broadcast_to(np.stack([ba[2048:3072], ba[5120:6144]], axis=0)[None], (128, 2, 1024)))
    m["n1g"] = f(np.asarray(inp["norm1_gain"])[0].reshape(8, 128).T)
    m["n2g"] = f(np.asarray(inp["norm2_gain"])[0].reshape(8, 128).T)
    m["w_in"] = f(inp["w_in"][0])
    qg_ = np.asarray(inp["attn_q_gain"])[0]; kg_ = np.asarray(inp["attn_k_gain"])[0]
    m["qkgain"] = f(np.stack([np.tile(qg_, 2), np.tile(kg_, 2)], axis=1))
    rb = np.asarray(inp["attn_rel_bias"])[0]
    kk = np.arange(128)[:, None]; qq = np.arange(128)[None, :]
    tabs = []
    for d in range(2):
        idx = np.clip(128 * d + qq - kk, -128, 128) + 128
        tabs.append(rb[:, idx])
    m["biasT"] = f(np.stack(tabs, axis=1).transpose(2, 0, 1, 3))
    m["cbias"] = rep(rb[:, 256])
    m["aog"] = rep(np.asarray(inp["attn_out_gain"])[0])
    m["convw"] = f(np.asarray(inp["dn_conv_w"])[0].reshape(4, 12, 128).transpose(2, 1, 0))
    m["alog"] = rep(np.asarray(inp["dn_a_log"])[0])
    m["dtb"] = rep(np.asarray(inp["dn_dt_bias"])[0])
    m["dog"] = rep(np.asarray(inp["dn_out_gain"])[0])
    m["w_out"] = f(inp["w_out"][0])
    m["wq"] = f(inp["peer_w_query"][0])
    m["qg"] = rep(np.asarray(inp["peer_query_gain"])[0])
    m["sk1T"] = f(np.asarray(inp["peer_sub_keys_1"])[0].T)
    m["sk2T"] = f(np.asarray(inp["peer_sub_keys_2"])[0].T)
    m["tabf"] = np.ascontiguousarray(np.concatenate([np.asarray(inp["peer_expert_down"][0], np.float32),
                                                     np.asarray(inp["peer_expert_up"][0], np.float32)], axis=1))
    m["cst"] = make_consts()
    m["iota16"] = np.ascontiguousarray(np.broadcast_to(np.arange(16, dtype=np.float32)[None, :], (128, 16)))
    return m


def kernel(**inputs):
    nc = build_program()
    shared = None
    in_maps = []
    for core in range(8):
        m = prep_inputs(inputs, core)
        if shared is None:
            shared = m
        else:
            for k in m:
                if k not in ("x", "cT"):
                    m[k] = shared[k]
        in_maps.append(m)
    res = run_bass_kernel_spmd(nc, in_maps, core_ids=list(range(8)))
    outs = [np.asarray(r["out"], dtype=np.float32).reshape(2, 2048, 1024) for r in res.results]
    return np.concatenate(outs, axis=0)
```

```python
from contextlib import ExitStack
import math
import numpy as np
import concourse.bass as bass
import concourse.mybir as mybir
from concourse.bass_utils import run_bass_kernel_spmd

F32 = mybir.dt.float32
BF16 = mybir.dt.bfloat16
I32 = mybir.dt.int32
U32 = mybir.dt.uint32
ALU = mybir.AluOpType
AF = mybir.ActivationFunctionType
AX = mybir.AxisListType

N_DMA_SEMS = 12
import os
KSTOP = int(os.environ.get('KSTOP', '99'))


class _Stop(Exception):
    pass


def chk(n):
    if KSTOP == n:
        raise _Stop()
EPS = 1e-6


class Sched:
    def __init__(self, nc, es):
        self.nc = nc
        self.eng = {"pe": nc.tensor, "act": nc.scalar, "dve": nc.vector,
                    "pool": nc.gpsimd, "sp": nc.sync}
        self.sem = {k: es.enter_context(nc.semaphore("s_" + k)) for k in self.eng}
        self.cnt = {k: 0 for k in self.eng}
        self.seen = {k: {} for k in self.eng}
        self.dsem = [es.enter_context(nc.semaphore("d%d" % i)) for i in range(N_DMA_SEMS)]
        self.dval = [0] * N_DMA_SEMS
        self.dnext = 0
        self.last_w = {}
        self.readers = {}
        self.n_wait = 0
        self.n_inst = 0

    def _wait(self, e, tok):
        kind, key, val = tok
        if kind == "e" and key == "pe" and e == "pe":
            return
        skey = (kind, key)
        if self.seen[e].get(skey, 0) >= val:
            return
        sem = self.sem[key] if kind == "e" else self.dsem[key]
        self.eng[e].wait_ge(sem, val)
        self.seen[e][skey] = val
        self.n_wait += 1

    def _deps(self, e, reads, writes):
        for b in reads:
            t = self.last_w.get(b)
            if t is not None:
                self._wait(e, t)
        for b in writes:
            t = self.last_w.get(b)
            if t is not None:
                self._wait(e, t)
            for t in self.readers.get(b, ()):
                self._wait(e, t)

    def _commit(self, tok, reads, writes):
        for b in reads:
            lst = self.readers.setdefault(b, [])
            lst.append(tok)
            if len(lst) > 16:
                newest = {}
                for t in lst:
                    k = (t[0], t[1])
                    if k not in newest or newest[k][2] < t[2]:
                        newest[k] = t
                self.readers[b] = list(newest.values())
        for b in writes:
            self.last_w[b] = tok
            self.readers[b] = []

    def op(self, e, fn, reads=(), writes=()):
        if e != "pe":
            extra = [r for r in reads if r.startswith("pb") or r.startswith("pq")]
            if extra:
                writes = list(writes) + extra
        self._deps(e, reads, writes)
        inst = fn(self.eng[e])
        self.cnt[e] += 1
        inst.then_inc(self.sem[e], 1)
        tok = ("e", e, self.cnt[e])
        self._commit(tok, reads, writes)
        self.n_inst += 1
        return tok

    def dma(self, q, fn, reads=(), writes=()):
        self._deps(q, reads, writes)
        i = self.dnext
        self.dnext = (self.dnext + 1) % N_DMA_SEMS
        if self.dval[i] > 0:
            self._wait(q, ("d", i, self.dval[i]))
        inst = fn(self.eng[q])
        self.dval[i] += 16
        inst.then_inc(self.dsem[i], 16)
        tok = ("d", i, self.dval[i])
        self._commit(tok, reads, writes)
        self.n_inst += 1
        return tok

    def barrier(self, engines=("pe", "act", "dve", "pool", "sp")):
        for e in engines:
            for k in self.eng:
                if self.cnt[k] and k != e:
                    self._wait(e, ("e", k, self.cnt[k]))
            for i in range(N_DMA_SEMS):
                if self.dval[i]:
                    self._wait(e, ("d", i, self.dval[i]))


def cv(ap, dims):
    return bass.AP(ap.tensor, ap.offset, [list(ap.ap[0])] + [list(d) for d in dims])


C_IDENT, C_TRI, C_TRIT, C_STRI, C_SELT, C_SEL0, C_SEL1, C_BLK64, C_ONES = range(9)
NCST = 9


def make_consts():
    c = np.zeros((128, NCST, 128), np.float32)
    i = np.arange(128)[:, None]
    j = np.arange(128)[None, :]
    same = (i // 64) == (j // 64)
    c[:, C_IDENT] = (i == j)
    c[:, C_TRI] = same & (j <= i)
    c[:, C_TRIT] = same & (i <= j)
    c[:, C_STRI] = same & (j < i)
    c[:, C_SELT] = (i == 64 * (j // 64) + 63)
    c[:, C_SEL0] = (i == 63)
    c[:, C_SEL1] = (i == 127)
    c[:, C_BLK64] = same / 64.0
    c[:, C_ONES] = 1.0
    return c


def build_program(NTILES=32, dbg=False, do_peer=True):
    nc = bass.Bass("TRN2", target_bir_lowering=False)
    D = {}

    def din(name, shape, dt=F32):
        D[name] = nc.dram_tensor(name, shape, dt, kind="ExternalInput").ap()

    din("x", [4096, 1024]); din("cT", [128, 8, 2]); din("w_ada", [1024, 6144])
    din("b_adaT", [128, 48]); din("b_gate", [128, 2, 1024]); din("n1g", [128, 8]); din("n2g", [128, 8])
    din("w_in", [1024, 3592]); din("qkgain", [128, 2]); din("biasT", [128, 8, 2, 128])
    din("cbias", [128, 8]); din("aog", [128, 512]); din("convw", [128, 12, 4])
    din("alog", [128, 4]); din("dtb", [128, 4]); din("dog", [128, 128])
    din("w_out", [1024, 1024]); din("wq", [1024, 2048]); din("qg", [128, 256])
    din("sk1T", [128, 128]); din("sk2T", [128, 128])
    din("tabf", [16384, 2048]); din("iota16", [128, 16]); din("cst", [128, NCST, 128])
    out = nc.dram_tensor("out", [4096, 1024], F32, kind="ExternalOutput").ap()
    tab = nc.dram_tensor("tab", [16384, 2048], BF16, kind="Internal").ap()
    if dbg:
        dbg_o = nc.dram_tensor("dbg", [4096, 1024], F32, kind="ExternalOutput").ap()

    with ExitStack() as es:
        S = Sched(nc, es)
        uid = [0]

        def sb(es_, name, shape, dt=F32):
            uid[0] += 1
            return es_.enter_context(nc.sbuf_tensor("%s_%d" % (name, uid[0]), shape, dt))

        PB = [es.enter_context(nc.psum_tensor("pb%d" % i, [128, 512], F32)) for i in range(8)]
        bank_rr = [0]

        def bank():
            i = bank_rr[0]
            bank_rr[0] = (i + 1) % 4
            return PB[i], "pb%d" % i

        q_rr = [0]

        def quarter():
            i = q_rr[0]
            q_rr[0] = (i + 1) % 6
            bi = (0, 1, 2, 3, 6, 7)[i]
            return PB[bi][:, 0:128], "pb%d" % bi

        cst = sb(es, "cst", [128, NCST, 128])
        S.dma("sp", lambda e: e.dma_start(out=cst[:], in_=D["cst"]), writes=["cst"])

        def C(i):
            return cst[:, i, :]
        identb = sb(es, "identb", [128, 128], BF16)
        S.op("dve", lambda e: e.tensor_copy(out=identb[:], in_=C(C_IDENT)), reads=["cst"], writes=["identb"])

        w_in_sb = sb(es, "w_in_sb", [128, 8, 3592], BF16)
        W_IN = []
        for k in range(8):
            for (c0, c1) in ((0, 2048), (2048, 3592)):
                nm = "w_in_%d_%d" % (k, c0)
                W_IN.append(nm)
                S.dma("pool", lambda e: e.dma_start(out=w_in_sb[:, k, c0:c1], in_=D["w_in"][k * 128:(k + 1) * 128, c0:c1]),
                      writes=[nm])
        w_out_sb = sb(es, "w_out_sb", [128, 8, 1024], BF16)
        wq_sb = sb(es, "wq_sb", [128, 8, 2048], BF16)
        W_OUT, W_Q = [], []
        for k in range(8):
            nm = "w_out_%d" % k
            W_OUT.append(nm)
            S.dma("pool", lambda e: e.dma_start(out=w_out_sb[:, k, :], in_=D["w_out"][k * 128:(k + 1) * 128, :]), writes=[nm])
            nm = "wq_%d" % k
            W_Q.append(nm)
            S.dma("pool", lambda e: e.dma_start(out=wq_sb[:, k, :], in_=D["wq"][k * 128:(k + 1) * 128, :]), writes=[nm])
        skT = sb(es, "skT", [128, 2, 128], BF16)
        S.dma("pool", lambda e: e.dma_start(out=skT[:, 0, :], in_=D["sk1T"]), writes=["sk1"])
        S.dma("pool", lambda e: e.dma_start(out=skT[:, 1, :], in_=D["sk2T"]), writes=["sk2"])

        small = sb(es, "small", [128, 2048])
        off = {}
        pos = [0]

        def sm_load(name, n, src, q="sp"):
            o = pos[0]
            off[name] = o
            pos[0] += n
            S.dma(q, lambda e: e.dma_start(out=small[:, o:o + n], in_=src), writes=["sm_" + name])
            return small[:, o:o + n]

        def sm_alloc(name, n):
            o = pos[0]
            off[name] = o
            pos[0] += n
            return small[:, o:o + n]

        b_adaT = sm_load("b_adaT", 48, D["b_adaT"])
        n1g = sm_load("n1g", 8, D["n1g"])
        n2g = sm_load("n2g", 8, D["n2g"])
        qkgain = sm_load("qkgain", 2, D["qkgain"])
        cbias = sm_load("cbias", 8, D["cbias"])
        convw = sm_load("convw", 48, D["convw"].rearrange("p a b -> p (a b)"))
        alog = sm_load("alog", 4, D["alog"])
        dtb = sm_load("dtb", 4, D["dtb"])
        dog = sm_load("dog", 128, D["dog"])
        qg = sm_load("qg", 256, D["qg"])
        aog = sm_load("aog", 512, D["aog"])
        iota16 = sm_load("iota16", 16, D["iota16"])
        nexpA = sm_alloc("nexpA", 4)
        modT = sm_alloc("modT", 96)
        A1 = sm_alloc("A1", 16); A2 = sm_alloc("A2", 16)
        gq = sm_alloc("gq", 1)
        assert pos[0] <= 2048
        S.op("act", lambda e: e.activation(out=nexpA, in_=alog, func=AF.Exp), reads=["sm_alog"], writes=["nexpA"])
        S.op("dve", lambda e: e.tensor_scalar(out=nexpA, in0=nexpA, scalar1=-1.0, scalar2=None, op0=ALU.mult),
             reads=["nexpA"], writes=["nexpA"])
        S.op("dve", lambda e: e.tensor_scalar(out=gq, in0=qkgain[:, 0:1], scalar1=0.125, scalar2=None, op0=ALU.mult),
             reads=["sm_qkgain"], writes=["gq"])

        expB = sb(es, "expB", [128, 8, 2, 128], BF16)
        gate_rows = sb(es, "gate_rows", [128, 2, 1024])
        csT = sb(es, "csT", [128, 8, 2])
        S.dma("sp", lambda e: e.dma_start(out=csT[:], in_=D["cT"]), writes=["csT"])
        S.op("act", lambda e: e.activation(out=csT[:], in_=csT[:], func=AF.Silu), reads=["csT"], writes=["csT"])

        x_sb = sb(es, "x_sb", [128, 1024])
        kT_ring = sb(es, "kT_ring", [128, 4, 640], BF16)
        V_ring = sb(es, "V_ring", [128, 5, 8, 65], BF16)
        S.op("pool", lambda e: e.memset(V_ring[:], 1.0), writes=["V_ring"])
        craw = sb(es, "craw", [128, 12, 131])
        Sst = sb(es, "Sst", [128, 4, 128])

        with ExitStack() as es0:
            bT = sb(es0, "bT", [128, 8 * 2 * 128])
            S.dma("sp", lambda e: e.dma_start(out=bT[:], in_=D["biasT"].rearrange("p a b c -> p (a b c)")), writes=["bT"])
            S.op("act", lambda e: e.activation(out=expB[:].rearrange("p a b c -> p (a b c)"), in_=bT[:], func=AF.Exp),
                 reads=["bT"], writes=["expB"])
            S.op("pool", lambda e: e.memset(expB[64:128, :, 0, 0:64], 0.0), reads=["expB"], writes=["expB"])

            wab = [sb(es0, "wab%d" % i, [128, 8, 512]) for i in range(2)]
            for blk in range(12):
                wb = wab[blk % 2]
                nm = "wab%d" % (blk % 2)
                S.dma("sp", lambda e: e.dma_start(out=wb[:], in_=D["w_ada"].rearrange("(k p) n -> p k n", p=128)[:, :, blk * 512:(blk + 1) * 512]),
                      writes=[nm])
                pq, pqn = quarter()
                for c4 in range(4):
                    for k in range(8):
                        S.op("pe", lambda e: e.matmul(pq[:, c4 * 2:c4 * 2 + 2], lhsT=wb[:, k, c4 * 128:(c4 + 1) * 128], rhs=csT[:, k, :],
                                                      start=(k == 0), stop=(k == 7)), reads=[nm, "csT"], writes=[pqn])
                o = off["modT"] + blk * 8
                S.op("dve", lambda e: e.tensor_tensor(out=cv(small[:, o:o + 1], [[2, 4], [1, 2]]),
                                                      in0=cv(pq[:, 0:1], [[2, 4], [1, 2]]),
                                                      in1=cv(small[:, off["b_adaT"] + blk * 4:off["b_adaT"] + blk * 4 + 1], [[1, 4], [0, 2]]),
                                                      op=ALU.add), reads=[pqn, "sm_b_adaT"], writes=["modT"])
            for (An, Anm, gn, gnm, ch0) in ((A1, "A1", n1g, "sm_n1g", 8), (A2, "A2", n2g, "sm_n2g", 32)):
                o = off["modT"] + ch0 * 2
                S.op("dve", lambda e: e.scalar_tensor_tensor(out=cv(An[:, 0:1], [[2, 8], [1, 2]]), in0=cv(small[:, o:o + 1], [[2, 8], [1, 2]]),
                                                             scalar=1.0, in1=cv(gn[:, 0:1], [[1, 8], [0, 2]]), op0=ALU.add, op1=ALU.mult),
                     reads=["modT", gnm], writes=[Anm])
            S.barrier()

        with ExitStack() as esc:
            cvb = [sb(esc, "cvb%d" % i, [128, 2, 2048], BF16) for i in range(4)]
            tabv = tab.rearrange("(p r) n -> p r n", p=128)
            srcv = D["tabf"].rearrange("(p r) n -> p r n", p=128)
            for c in range(64):
                cb_ = cvb[c % 4]; cn = "cvb%d" % (c % 4)
                S.dma("pool", lambda e: e.dma_start(out=cb_[:], in_=srcv[:, c * 2:(c + 1) * 2, :]), writes=[cn])
                S.dma("sp", lambda e: e.dma_start(out=tabv[:, c * 2:(c + 1) * 2, :], in_=cb_[:]), reads=[cn])
            S.barrier()

        def modv(ch, b):
            o = off["modT"] + ch * 2 + b
            return small[:, o:o + 1]

        def emit_gate_rows(b):
            with ExitStack() as esg:
                wab = [sb(esg, "wabg%d" % i, [128, 8, 512]) for i in range(2)]
                cb = sb(esg, "cb", [128, 8, 128])
                S.dma("sp", lambda e: e.dma_start(out=gate_rows[:], in_=D["b_gate"]), writes=["gate_rows"])
                for k in range(8):
                    S.op("dve", lambda e: e.tensor_copy(out=cb[:, k, :], in_=cv(csT[:, k, b:b + 1], [[0, 128]])), reads=["csT"], writes=["cb"])
                for gi, blk0 in ((0, 4), (1, 10)):
                    for hb in range(2):
                        blk = blk0 + hb
                        wb = wab[hb]
                        nm = "wabg%d" % hb
                        S.dma("sp", lambda e: e.dma_start(out=wb[:], in_=D["w_ada"].rearrange("(k p) n -> p k n", p=128)[:, :, blk * 512:(blk + 1) * 512]),
                              writes=[nm])
                        pb, pbn = bank()
                        for k in range(8):
                            S.op("pe", lambda e: e.matmul(pb[:, :], lhsT=cb[:, k, :], rhs=wb[:, k, :], start=(k == 0), stop=(k == 7)),
                                 reads=[nm, "cb"], writes=[pbn])
                        S.op("dve", lambda e: e.tensor_tensor(out=gate_rows[:, gi, hb * 512:(hb + 1) * 512], in0=pb[:, :],
                                                              in1=gate_rows[:, gi, hb * 512:(hb + 1) * 512], op=ALU.add),
                             reads=[pbn, "gate_rows"], writes=["gate_rows"])
                S.barrier()

        for ti in range(NTILES):
            b = ti // 16
            it = ti % 16
            if it == 0:
                emit_gate_rows(b)
                S.op("pool", lambda e: e.memset(Sst[:], 0.0), writes=["Sst0", "Sst1", "Sst2", "Sst3"])
            slot = it % 5
            S.dma("sp", lambda e: e.dma_start(out=x_sb[:], in_=D["x"][ti * 128:(ti + 1) * 128, :]), writes=["x"])
            if True:
              def mixer(em):
                _mixer_body = True
                xn = sb(em, "xn", [128, 1024])
                o_cat = sb(em, "o_cat", [128, 1024])
                st = sb(em, "st", [128, 64])
                hT = sb(em, "hT", [128, 8, 128], BF16)
                S.op("act", lambda e: e.activation(out=o_cat[:], in_=x_sb[:], func=AF.Square, accum_out=st[:, 0:1]), reads=["x"], writes=["ocs", "ss"])
                S.op("dve", lambda e: e.tensor_scalar(out=st[:, 1:2], in0=st[:, 0:1], scalar1=1.0 / 1024, scalar2=EPS, op0=ALU.mult, op1=ALU.add),
                     reads=["ss"], writes=["rs"])
                S.op("act", lambda e: e.activation(out=st[:, 1:2], in_=st[:, 1:2], func=AF.Sqrt), reads=["rs"], writes=["rs"])
                S.op("dve", lambda e: e.reciprocal(out=st[:, 1:2], in_=st[:, 1:2]), reads=["rs"], writes=["rs"])
                S.op("act", lambda e: e.activation(out=xn[:], in_=x_sb[:], func=AF.Copy, scale=st[:, 1:2]), reads=["x", "rs"], writes=["xn"])
                for half in range(2):
                    pb, pbn = bank()
                    for kk in range(4):
                        k = half * 4 + kk
                        S.op("pe", lambda e: e.transpose(out=pb[:, kk * 128:(kk + 1) * 128], in_=xn[:, k * 128:(k + 1) * 128], identity=C(C_IDENT)),
                             reads=["xn", "cst"], writes=[pbn])
                    for kk in range(4):
                        k = half * 4 + kk
                        a_ap = small[:, off["A1"] + k * 2 + b:off["A1"] + k * 2 + b + 1]
                        s_ap = modv(k, b)
                        if kk % 2 == 0:
                            S.op("dve", lambda e: e.tensor_scalar(out=hT[:, k, :], in0=pb[:, kk * 128:(kk + 1) * 128], scalar1=a_ap, scalar2=s_ap,
                                                                  op0=ALU.mult, op1=ALU.add), reads=[pbn, "A1", "modT"], writes=["hT"])
                        else:
                            S.op("act", lambda e: e.activation(out=hT[:, k, :], in_=pb[:, kk * 128:(kk + 1) * 128], func=AF.Identity, scale=a_ap, bias=s_ap),
                                 reads=[pbn, "A1", "modT"], writes=["hT"])

                chk(1)
                def proj_fm(pb, pbn, col0, nch):
                    for c4 in range(nch):
                        for k in range(8):
                            S.op("pe", lambda e: e.matmul(pb[:, c4 * 128:(c4 + 1) * 128], lhsT=w_in_sb[:, k, col0 + c4 * 128:col0 + (c4 + 1) * 128],
                                                          rhs=hT[:, k, :], start=(k == 0), stop=(k == 7)), reads=W_IN + ["hT"], writes=[pbn])

                def proj_tm(pb, pbn, col0, n):
                    for k in range(8):
                        S.op("pe", lambda e: e.matmul(pb[:, 0:n], lhsT=hT[:, k, :], rhs=w_in_sb[:, k, col0:col0 + n], start=(k == 0), stop=(k == 7)),
                             reads=W_IN + ["hT"], writes=[pbn])

                sqb = sb(em, "sqb", [128, 512])
                rb = sb(em, "rb", [128, 512])
                qT = sb(em, "qT", [128, 4, 128], BF16)
                for which in range(2):
                    pb, pbn = bank()
                    proj_fm(pb, pbn, which * 512, 4)
                    S.op("act", lambda e: e.activation(out=sqb[:], in_=pb[:, :], func=AF.Square), reads=[pbn], writes=["sqb"])
                    pm, pmn = bank()
                    S.op("pe", lambda e: e.matmul(pm[:, :], lhsT=C(C_BLK64), rhs=sqb[:], start=True, stop=True), reads=["cst", "sqb"], writes=[pmn])
                    S.op("dve", lambda e: e.tensor_scalar(out=rb[:], in0=pm[:, :], scalar1=EPS, scalar2=None, op0=ALU.add), reads=[pmn], writes=["rb"])
                    S.op("act", lambda e: e.activation(out=rb[:], in_=rb[:], func=AF.Sqrt), reads=["rb"], writes=["rb"])
                    S.op("dve", lambda e: e.reciprocal(out=rb[:], in_=rb[:]), reads=["rb"], writes=["rb"])
                    if which == 0:
                        S.op("dve", lambda e: e.scalar_tensor_tensor(out=qT[:].rearrange("p a b -> p (a b)"), in0=pb[:, :], scalar=gq, in1=rb[:],
                                                                     op0=ALU.mult, op1=ALU.mult), reads=[pbn, "rb", "gq"], writes=["qT"])
                    else:
                        for p4 in range(4):
                            S.op("dve", lambda e: e.scalar_tensor_tensor(out=kT_ring[:, p4, slot * 128:(slot + 1) * 128], in0=pb[:, p4 * 128:(p4 + 1) * 128],
                                                                         scalar=qkgain[:, 1:2], in1=rb[:, p4 * 128:(p4 + 1) * 128], op0=ALU.mult, op1=ALU.mult),
                                 reads=[pbn, "rb", "sm_qkgain"], writes=["kT_ring"])
                chk(2)
                pb, pbn = bank()
                proj_tm(pb, pbn, 1024, 512)
                S.op("act", lambda e: e.activation(out=V_ring[:, slot, :, 0:64], in_=pb[:, :].rearrange("p (a b) -> p a b", b=64), func=AF.Copy),
                     reads=[pbn], writes=["V_ring"])
                if it == 0:
                    S.op("pool", lambda e: e.memset(craw[:, :, 0:3], 0.0), writes=["craw"])
                else:
                    S.op("pool", lambda e: e.tensor_copy(out=craw[:, :, 0:3], in_=craw[:, :, 128:131]), reads=["craw"], writes=["craw"])
                for g3 in range(3):
                    pb, pbn = bank()
                    proj_fm(pb, pbn, 1536 + g3 * 512, 4)
                    S.op("act", lambda e: e.activation(out=craw[:, g3 * 4:(g3 + 1) * 4, 3:131], in_=pb[:, :].rearrange("p (a b) -> p a b", b=128), func=AF.Copy),
                         reads=[pbn], writes=["craw"])
                zs = sb(em, "zs", [128, 512])
                pb, pbn = bank()
                proj_tm(pb, pbn, 3072, 512)
                S.op("act", lambda e: e.activation(out=zs[:], in_=pb[:, :], func=AF.Silu), reads=[pbn], writes=["zs"])
                pb, pbn = bank()
                proj_tm(pb, pbn, 3584, 8)
                bet = st[:, 4:8]; gg = st[:, 8:12]
                S.op("act", lambda e: e.activation(out=bet, in_=pb[:, 0:4], func=AF.Sigmoid), reads=[pbn], writes=["bet"])
                S.op("dve", lambda e: e.tensor_tensor(out=gg, in0=pb[:, 4:8], in1=dtb, op=ALU.add), reads=[pbn, "sm_dtb"], writes=["gg"])
                S.op("act", lambda e: e.activation(out=gg, in_=gg, func=AF.Exp), reads=["gg"], writes=["gg"])
                S.op("act", lambda e: e.activation(out=gg, in_=gg, func=AF.Ln, bias=1.0), reads=["gg"], writes=["gg"])
                S.op("dve", lambda e: e.tensor_tensor(out=gg, in0=gg, in1=nexpA, op=ALU.mult), reads=["gg", "nexpA"], writes=["gg"])

                chk(3)
                Efs = [sb(em, "Ef%d" % i, [128, 256]) for i in range(2)]
                Eb = [sb(em, "Eb%d" % i, [128, 640], BF16) for i in range(2)]
                deltas = [d for d in range(5) if it - d >= 0]
                def attn_head(h):
                    p4, hf = h // 2, h % 2
                    pr = slice(64 * hf, 64 * hf + 64)
                    E = Eb[h % 2]; En = "Eb%d" % (h % 2)
                    Ef = Efs[h % 2]; Efn = "Ef%d" % (h % 2)
                    sa, san = bank()
                    sbk, sbn = bank()
                    for d in deltas:
                        sj = (it - d) % 5
                        dst = sa[:, d * 128:(d + 1) * 128] if d < 2 else sbk[:, (d - 2) * 128:(d - 1) * 128]
                        S.op("pe", lambda e: e.matmul(dst, lhsT=kT_ring[pr, p4, sj * 128:(sj + 1) * 128], rhs=qT[pr, p4, :], start=True, stop=True),
                             reads=["kT_ring", "qT"], writes=[san if d < 2 else sbn])
                    n01 = min(2, len(deltas))
                    S.op("act", lambda e: e.activation(out=Ef[:, 0:n01 * 128], in_=sa[:, 0:n01 * 128], func=AF.Exp), reads=[san], writes=[Efn])
                    S.op("dve", lambda e: e.tensor_tensor(out=E[:, 0:n01 * 128], in0=Ef[:, 0:n01 * 128],
                                                          in1=expB[:, h, 0:n01, :].rearrange("p a b -> p (a b)"), op=ALU.mult),
                         reads=[Efn, "expB"], writes=[En])
                    n2 = len(deltas) - 2
                    if n2 > 0:
                        S.op("act", lambda e: e.activation(out=E[:, 256:256 + n2 * 128], in_=sbk[:, 0:n2 * 128], func=AF.Exp, bias=cbias[:, h:h + 1]),
                             reads=[sbn, "sm_cbias"], writes=[En])
                        if n2 == 3:
                            S.op("pool", lambda e: e.memset(E[0:64, 512 + 64:640], 0.0), reads=[En], writes=[En])
                    yield
                    po = PB[4 + h // 4]; pon = "pb%d" % (4 + h // 4)
                    oc = (h % 4) * 65
                    for di, d in enumerate(deltas):
                        sj = (it - d) % 5
                        S.op("pe", lambda e: e.matmul(po[:, oc:oc + 65], lhsT=E[:, d * 128:(d + 1) * 128], rhs=V_ring[:, sj, h, :],
                                                      start=(di == 0), stop=(di == len(deltas) - 1)), reads=[En, "V_ring"], writes=[pon])
                    S.op("dve", lambda e: e.reciprocal(out=st[:, 16 + h:17 + h], in_=po[:, oc + 64:oc + 65]), reads=[pon], writes=["rinv%d" % h])
                    S.op("act", lambda e: e.activation(out=o_cat[:, h * 64:(h + 1) * 64], in_=po[:, oc:oc + 64], func=AF.Copy, scale=st[:, 16 + h:17 + h]),
                         reads=[pon, "rinv%d" % h], writes=["o_a%d" % h])
                for hp in range(0, 8, 2):
                    gens = [attn_head(hp + w_) for w_ in range(2)]
                    while gens:
                        for g_ in list(gens):
                            try:
                                next(g_)
                            except StopIteration:
                                gens.remove(g_)
                OA = ["o_a%d" % h for h in range(8)]

                chk(4)
                cs = sb(em, "cs", [128, 12, 128])
                acc = sb(em, "acc", [128, 128])
                for cc in range(12):
                    w0 = off["convw"] + cc * 4
                    S.op("dve", lambda e: e.tensor_scalar(out=acc[:], in0=craw[:, cc, 0:128], scalar1=small[:, w0:w0 + 1], scalar2=None, op0=ALU.mult),
                         reads=["craw", "sm_convw"], writes=["acc"])
                    for i3 in range(1, 4):
                        dst = cs[:, cc, :] if i3 == 3 else acc[:]
                        S.op("dve", lambda e: e.scalar_tensor_tensor(out=dst, in0=craw[:, cc, i3:i3 + 128], scalar=small[:, w0 + i3:w0 + i3 + 1], in1=acc[:],
                                                                     op0=ALU.mult, op1=ALU.add), reads=["craw", "sm_convw", "acc"],
                             writes=["cs"] if i3 == 3 else ["acc"])
                S.op("act", lambda e: e.activation(out=cs[:].rearrange("p a b -> p (a b)"), in_=cs[:].rearrange("p a b -> p (a b)"), func=AF.Silu),
                     reads=["cs"], writes=["cs"])
                for qk in range(2):
                    v4 = cs[:, qk * 4:(qk + 1) * 4, :].rearrange("p a b -> p (a b)")
                    S.op("act", lambda e: e.activation(out=sqb[:], in_=v4, func=AF.Square), reads=["cs"], writes=["sqb"])
                    pm, pmn = bank()
                    S.op("pe", lambda e: e.matmul(pm[:, :], lhsT=C(C_ONES), rhs=sqb[:], start=True, stop=True), reads=["cst", "sqb"], writes=[pmn])
                    S.op("dve", lambda e: e.tensor_scalar(out=rb[:], in0=pm[:, :], scalar1=EPS, scalar2=None, op0=ALU.add), reads=[pmn], writes=["rb"])
                    S.op("act", lambda e: e.activation(out=rb[:], in_=rb[:], func=AF.Sqrt), reads=["rb"], writes=["rb"])
                    S.op("dve", lambda e: e.reciprocal(out=rb[:], in_=rb[:]), reads=["rb"], writes=["rb"])
                    S.op("dve", lambda e: e.tensor_tensor(out=v4, in0=v4, in1=rb[:], op=ALU.mult), reads=["cs", "rb"], writes=["cs"])
                gcs = st[:, 24:28]; egq = st[:, 28:32]; egc = st[:, 32:36]; edec = st[:, 36:40]; egl = st[:, 40:48]; bge = st[:, 48:52]
                pq, pqn = quarter()
                S.op("pe", lambda e: e.matmul(pq[:, 0:4], lhsT=C(C_TRIT), rhs=gg, start=True, stop=True), reads=["cst", "gg"], writes=[pqn])
                S.op("dve", lambda e: e.tensor_copy(out=gcs, in_=pq[:, 0:4]), reads=[pqn], writes=["gcs"])
                S.op("act", lambda e: e.activation(out=egc, in_=gcs, func=AF.Exp), reads=["gcs"], writes=["egc"])
                S.op("dve", lambda e: e.tensor_scalar(out=egq, in0=egc, scalar1=128.0 ** -0.5, scalar2=None, op0=ALU.mult), reads=["egc"], writes=["egq"])
                S.op("dve", lambda e: e.tensor_tensor(out=bge, in0=egc, in1=bet, op=ALU.mult), reads=["egc", "bet"], writes=["bge"])
                pq, pqn = quarter()
                S.op("pe", lambda e: e.matmul(pq[:, 0:4], lhsT=C(C_SELT), rhs=gcs, start=True, stop=True), reads=["cst", "gcs"], writes=[pqn])
                S.op("pe", lambda e: e.matmul(pq[:, 4:8], lhsT=C(C_SEL0), rhs=gcs, start=True, stop=True), reads=["cst", "gcs"], writes=[pqn])
                S.op("pe", lambda e: e.matmul(pq[:, 8:12], lhsT=C(C_SEL1), rhs=gcs, start=True, stop=True), reads=["cst", "gcs"], writes=[pqn])
                S.op("dve", lambda e: e.tensor_tensor(out=edec, in0=pq[:, 0:4], in1=gcs, op=ALU.subtract), reads=[pqn, "gcs"], writes=["edec"])
                S.op("act", lambda e: e.activation(out=edec, in_=edec, func=AF.Exp), reads=["edec"], writes=["edec"])
                S.op("act", lambda e: e.activation(out=egl, in_=pq[:, 4:12], func=AF.Exp), reads=[pqn], writes=["egl"])

                chk(5)
                Gs = [{n: sb(em, "g" + str(w_) + "_" + n, [128, 128]) for n in ['gb', 'Dm', 'Ds', 'L', 'U', 'P0', 'P1', 'Lp', 'Lr0', 'Lr1', 'Ur0', 'Ur1', 'ktok', 'kbg', 'kdec', 'vbeta', 'wT', 'u', 'vnew', 'intra', 'intraT', 't1']} for w_ in range(2)]
                for w_ in range(2):
                    S.op("pool", lambda e: e.memset(Gs[w_]["vnew"][:], 0.0), writes=["vnew_" + str(w_)])
                GSET = set(['gb', 'Dm', 'Ds', 'L', 'U', 'P0', 'P1', 'Lp', 'Lr0', 'Lr1', 'Ur0', 'Ur1', 'ktok', 'kbg', 'kdec', 'vbeta', 'wT', 'u', 'vnew', 'intra', 'intraT', 't1'])

                def gdn_head(h, G, sfx):
                    def gop(e_, fn_, reads=(), writes=()):
                        def mp(n_):
                            if n_ in GSET:
                                return n_ + sfx
                            if n_ == "Sst":
                                return "Sst" + str(h)
                            return n_
                        return S.op(e_, fn_, reads=[mp(n_) for n_ in reads], writes=[mp(n_) for n_ in writes])
                    qTh = cs[:, h, :]; kTh = cs[:, 4 + h, :]; vTh = cs[:, 8 + h, :]
                    gop("dve", lambda e: e.tensor_copy(out=G["gb"][:], in_=cv(st[:, 8 + h:9 + h], [[0, 128]])), reads=["gg"], writes=["gb"])
                    yield
                    pg, pgn = quarter()
                    gop("pe", lambda e: e.matmul(pg, lhsT=G["gb"][:], rhs=C(C_TRIT), start=True, stop=True), reads=["gb", "cst"], writes=[pgn])
                    gop("dve", lambda e: e.tensor_scalar(out=G["Dm"][:], in0=pg, scalar1=gcs[:, h:h + 1], scalar2=0.0, op0=ALU.subtract, op1=ALU.max),
                         reads=[pgn, "gcs"], writes=["Dm"])
                    yield
                    gop("act", lambda e: e.activation(out=G["Dm"][:], in_=G["Dm"][:], func=AF.Exp, scale=-1.0), reads=["Dm"], writes=["Dm"])
                    yield
                    gop("dve", lambda e: e.tensor_tensor(out=G["Ds"][:], in0=G["Dm"][:], in1=C(C_STRI), op=ALU.mult), reads=["Dm", "cst"], writes=["Ds"])
                    yield
                    gop("dve", lambda e: e.tensor_tensor(out=G["Dm"][:], in0=G["Dm"][:], in1=C(C_TRI), op=ALU.mult), reads=["Dm", "cst"], writes=["Dm"])
                    yield
                    pk, pkn = quarter()
                    gop("act", lambda e: e.activation(out=G["gb"][:], in_=kTh, func=AF.Copy), reads=["cs"], writes=["gb"])
                    yield
                    gop("pe", lambda e: e.matmul(pk, lhsT=kTh, rhs=G["gb"][:], start=True, stop=True), reads=["cs", "gb"], writes=[pkn])
                    gop("dve", lambda e: e.scalar_tensor_tensor(out=G["L"][:], in0=pk, scalar=bet[:, h:h + 1], in1=G["Ds"][:], op0=ALU.mult, op1=ALU.mult),
                         reads=[pkn, "bet", "Ds"], writes=["L"])
                    yield
                    pqk, pqkn = quarter()
                    gop("pe", lambda e: e.matmul(pqk, lhsT=qTh, rhs=kTh, start=True, stop=True), reads=["cs"], writes=[pqkn])
                    gop("dve", lambda e: e.scalar_tensor_tensor(out=G["intra"][:], in0=pqk, scalar=128.0 ** -0.5, in1=G["Dm"][:], op0=ALU.mult, op1=ALU.mult),
                         reads=[pqkn, "Dm"], writes=["intra"])
                    yield
                    pt, ptn = quarter()
                    gop("pe", lambda e: e.transpose(out=pt, in_=G["L"][:], identity=C(C_IDENT)), reads=["L", "cst"], writes=[ptn])
                    gop("act", lambda e: e.activation(out=G["U"][:], in_=pt, func=AF.Copy), reads=[ptn], writes=["U"])
                    yield
                    gop("dve", lambda e: e.scalar_tensor_tensor(out=G["P0"][:], in0=pt, scalar=-1.0, in1=C(C_IDENT), op0=ALU.mult, op1=ALU.add), reads=[ptn, "cst", "U"], writes=["P0"])
                    yield
                    pt2, pt2n = quarter()
                    gop("pe", lambda e: e.transpose(out=pt2, in_=G["intra"][:], identity=C(C_IDENT)), reads=["intra", "cst"], writes=[pt2n])
                    gop("act", lambda e: e.activation(out=G["intraT"][:], in_=pt2, func=AF.Copy), reads=[pt2n], writes=["intraT"])
                    yield
                    pt3, pt3n = quarter()
                    gop("pe", lambda e: e.transpose(out=pt3, in_=kTh, identity=C(C_IDENT)), reads=["cs", "cst"], writes=[pt3n])
                    gop("dve", lambda e: e.tensor_scalar(out=G["kbg"][:], in0=pt3, scalar1=bge[:, h:h + 1], scalar2=None, op0=ALU.mult),
                         reads=[pt3n, "bge"], writes=["kbg"])
                    yield
                    gop("act", lambda e: e.activation(out=G["ktok"][:], in_=pt3, func=AF.Copy), reads=[pt3n], writes=["ktok"])
                    yield
                    pt4, pt4n = quarter()
                    gop("pe", lambda e: e.transpose(out=pt4, in_=vTh, identity=C(C_IDENT)), reads=["cs", "cst"], writes=[pt4n])
                    gop("act", lambda e: e.activation(out=G["vbeta"][:], in_=pt4, func=AF.Copy, scale=bet[:, h:h + 1]), reads=[pt4n, "bet"], writes=["vbeta"])
                    yield
                    Lc, Lcn, Uc, Ucn, Pc, Pcn = G["L"], "L", G["U"], "U", G["P0"], "P0"
                    for kk in range(1, 6):
                        pl, pln = quarter()
                        gop("pe", lambda e: e.matmul(pl, lhsT=Uc[:], rhs=Lc[:], start=True, stop=True), reads=[Ucn, Lcn], writes=[pln])
                        gop("dve", lambda e: e.tensor_tensor(out=G["Lp"][:], in0=pl, in1=C(C_IDENT), op=ALU.add), reads=[pln, "cst"], writes=["Lp"])
                        yield
                        if kk < 5:
                            pu, pun = quarter()
                            gop("pe", lambda e: e.matmul(pu, lhsT=Lc[:], rhs=Uc[:], start=True, stop=True), reads=[Ucn, Lcn], writes=[pun])
                            Ln_, Un_ = "Lr%d" % (kk % 2), "Ur%d" % (kk % 2)
                            gop("act", lambda e: e.activation(out=G[Ln_][:], in_=pl, func=AF.Copy), reads=[pln], writes=[Ln_])
                            yield
                            gop("act", lambda e: e.activation(out=G[Un_][:], in_=pu, func=AF.Copy), reads=[pun], writes=[Un_])
                            yield
                        pp, ppn = quarter()
                        gop("pe", lambda e: e.matmul(pp, lhsT=G["Lp"][:], rhs=Pc[:], start=True, stop=True), reads=["Lp", Pcn], writes=[ppn])
                        Pn_ = "P%d" % (kk % 2)
                        gop("dve", lambda e: e.tensor_copy(out=G[Pn_][:], in_=pp), reads=[ppn], writes=[Pn_])
                        yield
                        Pc, Pcn = G[Pn_], Pn_
                        if kk < 5:
                            Lc, Lcn, Uc, Ucn = G[Ln_], Ln_, G[Un_], Un_
                    pw, pwn = quarter()
                    gop("pe", lambda e: e.matmul(pw, lhsT=G["kbg"][:], rhs=Pc[:], start=True, stop=True), reads=["kbg", Pcn], writes=[pwn])
                    gop("act", lambda e: e.activation(out=G["wT"][:], in_=pw, func=AF.Copy), reads=[pwn], writes=["wT"])
                    yield
                    pu2, pu2n = quarter()
                    gop("pe", lambda e: e.matmul(pu2, lhsT=Pc[:], rhs=G["vbeta"][:], start=True, stop=True), reads=["vbeta", Pcn], writes=[pu2n])
                    gop("dve", lambda e: e.tensor_copy(out=G["u"][:], in_=pu2), reads=[pu2n], writes=["u"])
                    yield
                    Sh = Sst[:, h, :]
                    for c in range(2):
                        r = slice(64 * c, 64 * c + 64)
                        gop("dve", lambda e: e.tensor_scalar(out=G["kdec"][:], in0=G["ktok"][:], scalar1=edec[:, h:h + 1], scalar2=cst[:, C_TRI, 64 * c:64 * c + 1],
                                                              op0=ALU.mult, op1=ALU.mult), reads=["ktok", "edec", "cst"], writes=["kdec"])
                        yield
                        pa, pan = quarter()
                        gop("pe", lambda e: e.matmul(pa, lhsT=G["wT"][:], rhs=Sh, start=True, stop=True), reads=["wT", "Sst"], writes=[pan])
                        gop("dve", lambda e: e.scalar_tensor_tensor(out=G["vnew"][r, :], in0=pa[r, :], scalar=-1.0, in1=G["u"][r, :], op0=ALU.mult, op1=ALU.add),
                             reads=["u", pan], writes=["vnew"])
                        yield
                        po1, po1n = quarter()
                        gop("pe", lambda e: e.matmul(po1, lhsT=cs[:, h, :], rhs=Sh, start=True, stop=True), reads=["cs", "Sst"], writes=[po1n])
                        gop("act", lambda e: e.activation(out=G["t1"][r, :], in_=po1[r, :], func=AF.Copy, scale=egq[r, h:h + 1]), reads=[po1n, "egq"], writes=["t1"])
                        yield
                        po2, po2n = quarter()
                        gop("pe", lambda e: e.matmul(po2, lhsT=G["intraT"][:], rhs=G["vnew"][:], start=True, stop=True),
                             reads=["intraT", "vnew"], writes=[po2n])
                        gop("dve", lambda e: e.tensor_tensor(out=o_cat[r, 512 + h * 128:512 + (h + 1) * 128], in0=po2[r, :], in1=G["t1"][r, :], op=ALU.add),
                             reads=["t1", po2n], writes=["o_b%d" % h])
                        yield
                        psn, psnn = quarter()
                        gop("pe", lambda e: e.matmul(psn, lhsT=G["kdec"][:], rhs=G["vnew"][:], start=True, stop=True), reads=["kdec", "vnew"], writes=[psnn])
                        gop("dve", lambda e: e.scalar_tensor_tensor(out=Sh, in0=Sh, scalar=egl[:, c * 4 + h:c * 4 + h + 1], in1=psn, op0=ALU.mult, op1=ALU.add),
                             reads=["Sst", "egl", psnn], writes=["Sst"])
                        yield

                for hp in range(0, 4, 2):
                    gens = [gdn_head(hp + w_, Gs[w_], "_" + str(w_)) for w_ in range(2)]
                    while gens:
                        for g_ in list(gens):
                            try:
                                next(g_)
                            except StopIteration:
                                gens.remove(g_)
                OB = ["o_b%d" % h for h in range(4)]

                chk(6)
                catb = sb(em, "catb", [128, 1024], BF16)
                S.op("act", lambda e: e.activation(out=xn[:, 0:512], in_=o_cat[:, 0:512], func=AF.Square, accum_out=st[:, 52:53]), reads=OA, writes=["xn", "ssa"])
                S.op("dve", lambda e: e.tensor_scalar(out=st[:, 52:53], in0=st[:, 52:53], scalar1=1.0 / 512, scalar2=EPS, op0=ALU.mult, op1=ALU.add),
                     reads=["ssa"], writes=["ssa"])
                S.op("act", lambda e: e.activation(out=st[:, 52:53], in_=st[:, 52:53], func=AF.Sqrt), reads=["ssa"], writes=["ssa"])
                S.op("dve", lambda e: e.reciprocal(out=st[:, 52:53], in_=st[:, 52:53]), reads=["ssa"], writes=["ssa"])
                S.op("dve", lambda e: e.scalar_tensor_tensor(out=catb[:, 0:512], in0=o_cat[:, 0:512], scalar=st[:, 52:53], in1=aog, op0=ALU.mult, op1=ALU.mult),
                     reads=OA + ["ssa", "sm_aog"], writes=["catb"])
                for h in range(4):
                    sl = slice(512 + h * 128, 512 + (h + 1) * 128)
                    S.op("act", lambda e: e.activation(out=xn[:, 0:128], in_=o_cat[:, sl], func=AF.Square, accum_out=st[:, 56 + h:57 + h]),
                         reads=OB, writes=["xn", "ssb%d" % h])
                S.op("dve", lambda e: e.tensor_scalar(out=st[:, 56:60], in0=st[:, 56:60], scalar1=1.0 / 128, scalar2=EPS, op0=ALU.mult, op1=ALU.add),
                     reads=["ssb%d" % h for h in range(4)], writes=["ssb"])
                S.op("act", lambda e: e.activation(out=st[:, 56:60], in_=st[:, 56:60], func=AF.Sqrt), reads=["ssb"], writes=["ssb"])
                S.op("dve", lambda e: e.reciprocal(out=st[:, 56:60], in_=st[:, 56:60]), reads=["ssb"], writes=["ssb"])
                for h in range(4):
                    sl = slice(512 + h * 128, 512 + (h + 1) * 128)
                    S.op("dve", lambda e: e.scalar_tensor_tensor(out=xn[:, 0:128], in0=o_cat[:, sl], scalar=st[:, 56 + h:57 + h], in1=dog, op0=ALU.mult, op1=ALU.mult),
                         reads=OB + ["ssb", "sm_dog"], writes=["xn"])
                    S.op("dve", lambda e: e.tensor_tensor(out=catb[:, sl], in0=xn[:, 0:128], in1=zs[:, h * 128:(h + 1) * 128], op=ALU.mult),
                         reads=["xn", "zs"], writes=["catb"])
                cTb = sb(em, "cTb", [128, 8, 128], BF16)
                pb, pbn = bank()
                pbb = pb[:, :].bitcast(BF16)
                for k in range(8):
                    S.op("pe", lambda e: e.transpose(out=pbb[:, k * 128:(k + 1) * 128], in_=catb[:, k * 128:(k + 1) * 128], identity=identb[:]),
                         reads=["catb", "identb"], writes=[pbn])
                S.op("act", lambda e: e.activation(out=cTb[:].rearrange("p a b -> p (a b)"), in_=pbb, func=AF.Copy), reads=[pbn], writes=["cTb"])
                for nh in range(2):
                    pb, pbn = bank()
                    for k in range(8):
                        S.op("pe", lambda e: e.matmul(pb[:, :], lhsT=cTb[:, k, :], rhs=w_out_sb[:, k, nh * 512:(nh + 1) * 512], start=(k == 0), stop=(k == 7)),
                             reads=["cTb"] + W_OUT, writes=[pbn])
                    S.op("dve", lambda e: e.tensor_tensor(out=xn[:, nh * 512:(nh + 1) * 512], in0=pb[:, :], in1=gate_rows[:, 0, nh * 512:(nh + 1) * 512], op=ALU.mult),
                         reads=[pbn, "gate_rows"], writes=["xn"])
                    S.op("dve", lambda e: e.tensor_tensor(out=x_sb[:, nh * 512:(nh + 1) * 512], in0=x_sb[:, nh * 512:(nh + 1) * 512], in1=xn[:, nh * 512:(nh + 1) * 512], op=ALU.add),
                         reads=["x", "xn"], writes=["x"])
                if dbg:
                    S.dma("sp", lambda e: e.dma_start(out=dbg_o[ti * 128:(ti + 1) * 128, :], in_=x_sb[:]), reads=["x"])
                S.barrier()
              with ExitStack() as em:
                  try:
                      mixer(em)
                  except _Stop:
                      S.barrier()

            if do_peer:
                with ExitStack() as ep:
                    xn = sb(ep, "xn2", [128, 1024])
                    junk = sb(ep, "junk2", [128, 1024])
                    st = sb(ep, "st2", [128, 64])
                    h2Tb = sb(ep, "h2Tb", [128, 8, 128], BF16)
                    h2 = sb(ep, "h2", [128, 1024], BF16)
                    S.op("act", lambda e: e.activation(out=junk[:], in_=x_sb[:], func=AF.Square, accum_out=st[:, 0:1]), reads=["x"], writes=["junk", "ss"])
                    S.op("dve", lambda e: e.tensor_scalar(out=st[:, 1:2], in0=st[:, 0:1], scalar1=1.0 / 1024, scalar2=EPS, op0=ALU.mult, op1=ALU.add),
                         reads=["ss"], writes=["rs"])
                    S.op("act", lambda e: e.activation(out=st[:, 1:2], in_=st[:, 1:2], func=AF.Sqrt), reads=["rs"], writes=["rs"])
                    S.op("dve", lambda e: e.reciprocal(out=st[:, 1:2], in_=st[:, 1:2]), reads=["rs"], writes=["rs"])
                    S.op("act", lambda e: e.activation(out=xn[:], in_=x_sb[:], func=AF.Copy, scale=st[:, 1:2]), reads=["x", "rs"], writes=["xn"])
                    for half in range(2):
                        pb, pbn = bank()
                        for kk in range(4):
                            k = half * 4 + kk
                            S.op("pe", lambda e: e.transpose(out=pb[:, kk * 128:(kk + 1) * 128], in_=xn[:, k * 128:(k + 1) * 128], identity=C(C_IDENT)),
                                 reads=["xn", "cst"], writes=[pbn])
                        for kk in range(4):
                            k = half * 4 + kk
                            a_ap = small[:, off["A2"] + k * 2 + b:off["A2"] + k * 2 + b + 1]
                            s_ap = modv(24 + k, b)
                            S.op("dve", lambda e: e.tensor_scalar(out=h2Tb[:, k, :], in0=pb[:, kk * 128:(kk + 1) * 128], scalar1=a_ap, scalar2=s_ap,
                                                                  op0=ALU.mult, op1=ALU.add), reads=[pbn, "A2", "modT"], writes=["h2Tb"])
                    pb, pbn = bank()
                    pbb = pb[:, :].bitcast(BF16)
                    for k in range(8):
                        S.op("pe", lambda e: e.transpose(out=pbb[:, k * 128:(k + 1) * 128], in_=h2Tb[:, k, :], identity=identb[:]),
                             reads=["h2Tb", "identb"], writes=[pbn])
                    S.op("act", lambda e: e.activation(out=h2[:], in_=pbb, func=AF.Copy), reads=[pbn], writes=["h2"])
                    qn = sb(ep, "qn", [128, 2048], BF16)
                    qbanks = []
                    QSS = ["qss%d" % h for h in range(8)]
                    for nb in range(4):
                        pb, pbn = bank()
                        qbanks.append((pb, pbn))
                        for k in range(8):
                            S.op("pe", lambda e: e.matmul(pb[:, :], lhsT=h2Tb[:, k, :], rhs=wq_sb[:, k, nb * 512:(nb + 1) * 512], start=(k == 0), stop=(k == 7)),
                                 reads=["h2Tb"] + W_Q, writes=[pbn])
                        for hh in range(2):
                            hq = nb * 2 + hh
                            jr = (hq % 4) * 256
                            S.op("act", lambda e: e.activation(out=junk[:, jr:jr + 256], in_=pb[:, hh * 256:(hh + 1) * 256], func=AF.Square, accum_out=st[:, 8 + hq:9 + hq]),
                                 reads=[pbn], writes=["junk%d" % (hq % 4), "qss%d" % hq])
                    S.op("dve", lambda e: e.tensor_scalar(out=st[:, 8:16], in0=st[:, 8:16], scalar1=1.0 / 256, scalar2=EPS, op0=ALU.mult, op1=ALU.add),
                         reads=QSS, writes=["qssall"] + QSS)
                    S.op("act", lambda e: e.activation(out=st[:, 8:16], in_=st[:, 8:16], func=AF.Sqrt), reads=["qssall"], writes=["qssall"])
                    S.op("dve", lambda e: e.reciprocal(out=st[:, 8:16], in_=st[:, 8:16]), reads=["qssall"], writes=["qssall"])
                    for nb in range(4):
                        pb, pbn = qbanks[nb]
                        for hh in range(2):
                            hq = nb * 2 + hh
                            S.op("dve", lambda e: e.scalar_tensor_tensor(out=qn[:, hq * 256:(hq + 1) * 256], in0=pb[:, hh * 256:(hh + 1) * 256], scalar=st[:, 8 + hq:9 + hq],
                                                                         in1=qg, op0=ALU.mult, op1=ALU.mult), reads=[pbn, "qssall", "sm_qg"], writes=["qn%d" % hq])
                    QN = ["qn%d" % h for h in range(8)]
                    qnT = sb(ep, "qnT", [128, 16, 128], BF16)
                    for half in range(2):
                        pb, pbn = bank()
                        pbb = pb[:, :].bitcast(BF16)
                        for kk in range(8):
                            k = half * 8 + kk
                            S.op("pe", lambda e: e.transpose(out=pbb[:, kk * 128:(kk + 1) * 128], in_=qn[:, k * 128:(k + 1) * 128], identity=identb[:]),
                                 reads=QN + ["identb"], writes=[pbn])
                        S.op("act", lambda e: e.activation(out=qnT[:, half * 8:(half + 1) * 8, :].rearrange("p a b -> p (a b)"), in_=pbb, func=AF.Copy),
                             reads=[pbn], writes=["qnT"])
                    W = 2
                    ssc = [sb(ep, "ssc%d" % i, [128, 256]) for i in range(W)]
                    ssr = [sb(ep, "ssr%d" % i, [128, 128]) for i in range(W)]
                    mx = [sb(ep, "mx%d" % i, [128, 32]) for i in range(W)]
                    mi = [sb(ep, "mi%d" % i, [128, 32], U32) for i in range(W)]
                    cand_s = [sb(ep, "cand_s%d" % i, [128, 256]) for i in range(W)]
                    cand_r = [sb(ep, "cand_r%d" % i, [128, 256]) for i in range(W)]
                    mif = sb(ep, "mif", [128, 8, 32])
                    ts = sb(ep, "ts", [128, 8, 16])
                    posa = sb(ep, "posa", [128, 8, 16], U32)
                    pab = sb(ep, "pab", [128, 2, 128], U32)
                    pabf = sb(ep, "pabf", [128, 2, 128])
                    isel = sb(ep, "isel", [128, 2, 128])
                    eid = sb(ep, "eid", [128, 128])
                    eidi = sb(ep, "eidi", [128, 128], I32)

                    def head_chain(hq, w):
                        sc = ssc[w]; scn = "ssc%d" % w
                        pb, pbn = bank()
                        for hf in range(2):
                            S.op("pe", lambda e: e.matmul(pb[:, hf * 128:(hf + 1) * 128], lhsT=qnT[:, hq * 2 + hf, :], rhs=skT[:, hf, :], start=True, stop=True),
                                 reads=["qnT", "sk1", "sk2"], writes=[pbn])
                        S.op("act", lambda e: e.activation(out=sc[:], in_=pb[:, 0:256], func=AF.Copy), reads=[pbn], writes=[scn])
                        yield
                        mxn, min_, srn = "mx%d" % w, "mi%d" % w, "ssr%d" % w
                        for hf in range(2):
                            s_ = sc[:, hf * 128:(hf + 1) * 128]
                            m0 = mx[w][:, hf * 16:hf * 16 + 8]; m1 = mx[w][:, hf * 16 + 8:hf * 16 + 16]
                            S.op("dve", lambda e: e.max(out=m0, in_=s_), reads=[scn], writes=[mxn]); yield
                            S.op("dve", lambda e: e.max_index(out=mi[w][:, hf * 16:hf * 16 + 8], in_max=m0, in_values=s_), reads=[scn, mxn], writes=[min_]); yield
                            S.op("dve", lambda e: e.match_replace(out=ssr[w][:], in_to_replace=m0, in_values=s_, imm_value=-1e30), reads=[scn, mxn], writes=[srn]); yield
                            S.op("dve", lambda e: e.max(out=m1, in_=ssr[w][:]), reads=[srn], writes=[mxn]); yield
                            S.op("dve", lambda e: e.max_index(out=mi[w][:, hf * 16 + 8:hf * 16 + 16], in_max=m1, in_values=ssr[w][:]), reads=[srn, mxn], writes=[min_]); yield
                        S.op("dve", lambda e: e.tensor_copy(out=mif[:, hq, :], in_=mi[w][:]), reads=[min_], writes=["mif%d" % hq]); yield
                        csn, crn = "cand_s%d" % w, "cand_r%d" % w
                        S.op("dve", lambda e: e.tensor_tensor(out=cand_s[w][:].rearrange("p (a b) -> p a b", b=16), in0=cv(mx[w][:, 0:1], [[1, 16], [0, 16]]),
                                                              in1=cv(mx[w][:, 16:17], [[0, 16], [1, 16]]), op=ALU.add), reads=[mxn], writes=[csn]); yield
                        S.op("dve", lambda e: e.max(out=ts[:, hq, 0:8], in_=cand_s[w][:]), reads=[csn], writes=["ts%d" % hq]); yield
                        S.op("dve", lambda e: e.match_replace(out=cand_r[w][:], in_to_replace=ts[:, hq, 0:8], in_values=cand_s[w][:], imm_value=-1e30),
                             reads=[csn, "ts%d" % hq], writes=[crn]); yield
                        S.op("dve", lambda e: e.max(out=ts[:, hq, 8:16], in_=cand_r[w][:]), reads=[crn], writes=["ts%d" % hq]); yield
                        S.op("dve", lambda e: e.max_index(out=posa[:, hq, 0:8], in_max=ts[:, hq, 0:8], in_values=cand_s[w][:]), reads=[csn, "ts%d" % hq], writes=["pos%d" % hq]); yield
                        S.op("dve", lambda e: e.max_index(out=posa[:, hq, 8:16], in_max=ts[:, hq, 8:16], in_values=cand_r[w][:]), reads=[crn, "ts%d" % hq], writes=["pos%d" % hq]); yield

                    for h0 in range(0, 8, W):
                        gens = [head_chain(h0 + w, w) for w in range(W)]
                        while gens:
                            for g_ in list(gens):
                                try:
                                    next(g_)
                                except StopIteration:
                                    gens.remove(g_)
                    TS = ["ts%d" % h for h in range(8)]
                    POS = ["pos%d" % h for h in range(8)]
                    MIF = ["mif%d" % h for h in range(8)]
                    posf = posa[:].rearrange("p a b -> p (a b)")
                    S.op("dve", lambda e: e.tensor_single_scalar(out=pab[:, 0, :], in_=posf, scalar=4, op=ALU.logical_shift_right), reads=POS, writes=["pab0"])
                    S.op("dve", lambda e: e.tensor_single_scalar(out=pab[:, 1, :], in_=posf, scalar=15, op=ALU.bitwise_and), reads=POS, writes=["pab1"])
                    S.op("dve", lambda e: e.tensor_copy(out=pabf[:].rearrange("p a b -> p (a b)"), in_=pab[:].rearrange("p a b -> p (a b)")), reads=["pab0", "pab1"], writes=["pabf"])
                    for g4 in range(2):
                        for w_ in range(2):
                            scr = junk[:, (w_ * 1024) % 1024:(w_ * 1024) % 1024 + 1024].rearrange("p (h k a) -> p h k a", h=4, k=16)
                            scn_ = "junk"
                            S.op("dve", lambda e: e.tensor_tensor(out=scr, in0=cv(pabf[:, w_, g4 * 64:g4 * 64 + 1], [[16, 4], [1, 16], [0, 16]]),
                                                                  in1=cv(iota16[:, 0:1], [[0, 4], [0, 16], [1, 16]]), op=ALU.is_equal),
                                 reads=["pabf", "sm_iota16"], writes=["junk0", "junk1", "junk2", "junk3"])
                            S.op("dve", lambda e: e.tensor_tensor(out=scr, in0=scr, in1=cv(mif[:, g4 * 4, w_ * 16:w_ * 16 + 1], [[32, 4], [0, 16], [1, 16]]), op=ALU.mult),
                                 reads=["junk0"] + MIF, writes=["junk0", "junk1", "junk2", "junk3"])
                            S.op("dve", lambda e: e.tensor_reduce(out=isel[:, w_, g4 * 64:(g4 + 1) * 64].rearrange("p (h k) -> p h k", k=16), in_=scr, axis=AX.X, op=ALU.add),
                                 reads=["junk0"], writes=["isel%d%d" % (w_, g4)])
                    S.op("dve", lambda e: e.scalar_tensor_tensor(out=eid[:], in0=isel[:, 0, :], scalar=128.0, in1=isel[:, 1, :], op0=ALU.mult, op1=ALU.add),
                         reads=["isel00", "isel01", "isel10", "isel11"], writes=["eid"])
                    S.op("dve", lambda e: e.memset(junk[:, 0:8], 0.0), reads=["eid"], writes=["junk0", "eid"])
                    S.op("dve", lambda e: e.tensor_copy(out=eidi[:], in_=eid[:]), reads=["eid"], writes=["eidi"])
                    ge = sb(ep, "ge", [128, 8, 16])
                    S.op("dve", lambda e: e.tensor_scalar(out=st[:, 24:32], in0=cv(ts[:, 0, 0:1], [[16, 8]]), scalar1=-1.0, scalar2=None, op0=ALU.mult),
                         reads=TS, writes=["nts0"])
                    for hq in range(8):
                        S.op("act", lambda e: e.activation(out=ge[:, hq, :], in_=ts[:, hq, :], func=AF.Exp, bias=st[:, 24 + hq:25 + hq], accum_out=st[:, 32 + hq:33 + hq]),
                             reads=["ts%d" % hq, "nts0"], writes=["ge", "zs%d" % hq])
                    S.op("dve", lambda e: e.reciprocal(out=st[:, 32:40], in_=st[:, 32:40]), reads=["zs%d" % h for h in range(8)], writes=["rz"])
                    S.op("dve", lambda e: e.tensor_tensor(out=ge[:], in0=ge[:], in1=cv(st[:, 32:33], [[1, 8], [0, 16]]), op=ALU.mult), reads=["ge", "rz"], writes=["ge"])
                    NB = 4
                    uv = [sb(ep, "uv%d" % i, [128, 2048], BF16)[:] for i in range(NB)]
                    uvn = [["uv%d" % i] for i in range(NB)]
                    uv += [qn[:], qnT[:].rearrange("p a b -> p (a b)"), xn[:].bitcast(BF16), junk[:].bitcast(BF16)]
                    uvn += [QN, ["qnT"], ["xn"], ["junk", "junk0", "junk1", "junk2", "junk3"]]
                    NB = len(uv)
                    dg = [sb(ep, "dg%d" % i, [128, 128], BF16) for i in range(NB)]
                    pre = sb(ep, "pre", [128, 128])
                    gl = sb(ep, "gl", [128, 128])
                    wgt = sb(ep, "wgt", [128, 128])
                    S.op("dve", lambda e: e.memset(pre[:], 0.0), writes=["pre"])
                    gef = ge[:].rearrange("p a b -> p (a b)")
                    def consume(j):
                        u_ = uv[j % NB]; un = uvn[j % NB]
                        d_ = dg[j % NB]; dn = "dg%d" % (j % NB)
                        S.op("act", lambda e: e.activation(out=gl[:, j:j + 1], in_=pre[:, j:j + 1], func=AF.Gelu), reads=["pre%d" % j, "pre%d" % (j + 1)], writes=["gl%d" % j])
                        S.op("act", lambda e: e.activation(out=wgt[:, j:j + 1], in_=gl[:, j:j + 1], func=AF.Copy, scale=gef[:, j:j + 1]), reads=["gl%d" % j, "ge"], writes=["w%d" % j])
                        S.op("act", lambda e: e.activation(out=d_[:], in_=identb[:], func=AF.Copy, scale=wgt[:, j:j + 1]), reads=["identb", "w%d" % j], writes=[dn])
                        for nh in range(2):
                            S.op("pe", lambda e: e.matmul(PB[4 + nh][:, :], lhsT=d_[:], rhs=u_[:, 1024 + nh * 512:1024 + (nh + 1) * 512], start=(j == 0), stop=(j == 127)),
                                 reads=[dn] + un, writes=["pb%d" % (4 + nh)])

                    for j in range(128):
                        u_ = uv[j % NB]; un = uvn[j % NB]
                        S.dma("pool", lambda e: e.indirect_dma_start(out=u_, out_offset=None, in_=tab,
                                                                     in_offset=bass.IndirectOffsetOnAxis(ap=eidi[:, j:j + 1], axis=0)),
                              reads=["eidi"], writes=un)
                        S.op("dve", lambda e: e.scalar_tensor_tensor(out=h2Tb[:].rearrange("p a b -> p (a b)"), in0=u_[:, 0:1024], scalar=1.0, in1=h2[:], op0=ALU.mult, op1=ALU.mult,
                                                                     accum_out=pre[:, j:j + 1]), reads=un + ["h2"], writes=["h2Tb", "pre%d" % j])
                        if j >= 1:
                            consume(j - 1)
                    S.op("dve", lambda e: e.memset(st[:, 60:61], 0.0), writes=["pre128"])
                    consume(127)
                    for nh in range(2):
                        S.op("dve", lambda e: e.tensor_tensor(out=junk[:, nh * 512:(nh + 1) * 512], in0=PB[4 + nh][:, :], in1=gate_rows[:, 1, nh * 512:(nh + 1) * 512], op=ALU.mult),
                             reads=["pb%d" % (4 + nh), "gate_rows"], writes=["junk", "junk0", "junk1", "junk2", "junk3"])
                        S.op("dve", lambda e: e.tensor_tensor(out=x_sb[:, nh * 512:(nh + 1) * 512], in0=x_sb[:, nh * 512:(nh + 1) * 512], in1=junk[:, nh * 512:(nh + 1) * 512], op=ALU.add),
                             reads=["x", "junk"], writes=["x"])
                    S.dma("sp", lambda e: e.dma_start(out=out[ti * 128:(ti + 1) * 128, :], in_=x_sb[:]), reads=["x"])
                    S.barrier()
            else:
                S.dma("sp", lambda e: e.dma_start(out=out[ti * 128:(ti + 1) * 128, :], in_=x_sb[:]), reads=["x"])
                S.barrier()
        S.barrier(engines=("sp",))
        print("instructions", S.n_inst, "waits", S.n_wait, S.cnt, S.dval)
    return nc


def prep_inputs(inp, core):
    f = lambda a: np.ascontiguousarray(np.asarray(a, dtype=np.float32))
    b0 = core * 2
    rep = lambda v: f(np.broadcast_to(np.asarray(v, np.float32).reshape(1, -1), (128, np.asarray(v).size)))
    m = {}
    m["x"] = f(inp["x"][b0:b0 + 2].reshape(4096, 1024))
    m["cT"] = f(np.asarray(inp["c"])[b0:b0 + 2].reshape(2, 8, 128).transpose(2, 1, 0))
    m["w_ada"] = f(inp["w_ada"][0])
    m["b_adaT"] = f(np.asarray(inp["b_ada"])[0].reshape(48, 128).T)
    ba = np.asarray(inp["b_ada"])[0]
    m["b_gate"] = f(np.broadcast_to(np.stack([ba[2048:3072], ba[5120:6144]], axis=0)[None], (128, 2, 1024)))
    m["n1g"] = f(np.asarray(inp["norm1_gain"])[0].reshape(8, 128).T)
    m["n2g"] = f(np.asarray(inp["norm2_gain"])[0].reshape(8, 128).T)
    m["w_in"] = f(inp["w_in"][0])
    qg_ = np.asarray(inp["attn_q_gain"])[0]; kg_ = np.asarray(inp["attn_k_gain"])[0]
    m["qkgain"] = f(np.stack([np.tile(qg_, 2), np.tile(kg_, 2)], axis=1))
    rb = np.asarray(inp["attn_rel_bias"])[0]
    kk = np.arange(128)[:, None]; qq = np.arange(128)[None, :]
    tabs = []
    for d in range(2):
        idx = np.clip(128 * d + qq - kk, -128, 128) + 128
        tabs.append(rb[:, idx])
    m["biasT"] = f(np.stack(tabs, axis=1).transpose(2, 0, 1, 3))
    m["cbias"] = rep(rb[:, 256])
    m["aog"] = rep(np.asarray(inp["attn_out_gain"])[0])
    m["convw"] = f(np.asarray(inp["dn_conv_w"])[0].reshape(4, 12, 128).transpose(2, 1, 0))
    m["alog"] = rep(np.asarray(inp["dn_a_log"])[0])
    m["dtb"] = rep(np.asarray(inp["dn_dt_bias"])[0])
    m["dog"] = rep(np.asarray(inp["dn_out_gain"])[0])
    m["w_out"] = f(inp["w_out"][0])
    m["wq"] = f(inp["peer_w_query"][0])
    m["qg"] = rep(np.asarray(inp["peer_query_gain"])[0])
    m["sk1T"] = f(np.asarray(inp["peer_sub_keys_1"])[0].T)
    m["sk2T"] = f(np.asarray(inp["peer_sub_keys_2"])[0].T)
    m["tabf"] = np.ascontiguousarray(np.concatenate([np.asarray(inp["peer_expert_down"][0], np.float32),
                                                     np.asarray(inp["peer_expert_up"][0], np.float32)], axis=1))
    m["cst"] = make_consts()
    m["iota16"] = np.ascontiguousarray(np.broadcast_to(np.arange(16, dtype=np.float32)[None, :], (128, 16)))
    return m


def kernel(**inputs):
    nc = build_program()
    shared = None
    in_maps = []
    for core in range(8):
        m = prep_inputs(inputs, core)
        if shared is None:
            shared = m
        else:
            for k in m:
                if k not in ("x", "cT"):
                    m[k] = shared[k]
        in_maps.append(m)
    res = run_bass_kernel_spmd(nc, in_maps, core_ids=list(range(8)))
    outs = [np.asarray(r["out"], dtype=np.float32).reshape(2, 2048, 1024) for r in res.results]
    return np.concatenate(outs, axis=0)
```
